# Optimizing a Trainium2 kernel written in Bass

```python
import jax, jax.numpy as jnp
from jax import lax
import numpy as np

D_MODEL = 2048
BATCH = 16
SEQ = 2048
DEPTH = 1

D_MIX = D_MODEL
RET_HEADS = 8
RET_HEAD_DIM = 128
RET_WIDTH = RET_HEADS * RET_HEAD_DIM
RET_CHUNK = 128
ROPE_BASE = 10000.0
LRU_WIDTH = D_MIX - RET_WIDTH
LRU_BLOCKS = 8
LRU_BLOCK_DIM = LRU_WIDTH // LRU_BLOCKS
CONV_WIDTH = 4
LRU_C = 8.0
PROJ_IN = 4 * RET_WIDTH + 2 * LRU_WIDTH
N_GROUPS = 4
EXPERTS_PER_GROUP = 8
N_EXPERTS = N_GROUPS * EXPERTS_PER_GROUP
TOP_K = 2
D_EXPERT = D_MODEL // 2
MOE_BLOCK = 128
EPS = 1e-6

kernel_name = "hymba_retention_rglru_hmoe"


def rms_norm(x, g):
    xf = x.astype(jnp.float32)
    y = xf * lax.rsqrt(jnp.mean(xf * xf, axis=-1, keepdims=True) + EPS)
    return (y * g.astype(jnp.float32)).astype(x.dtype)


def rope(x, pos):
    half = x.shape[-1] // 2
    inv = ROPE_BASE ** (-jnp.arange(half, dtype=jnp.float32) / half)
    ang = pos.astype(jnp.float32)[:, None] * inv[None, :]
    cos = jnp.cos(ang)[None, :, None, :]
    sin = jnp.sin(ang)[None, :, None, :]
    xf = x.astype(jnp.float32)
    x1, x2 = xf[..., :half], xf[..., half:]
    return jnp.concatenate([x1 * cos - x2 * sin, x1 * sin + x2 * cos], axis=-1).astype(x.dtype)


def retention_chunkwise(q, k, v):
    B, S, H, d = q.shape
    C = RET_CHUNK
    N = S // C
    log_g = jnp.log1p(-(2.0 ** (-5.0 - jnp.arange(H, dtype=jnp.float32))))
    idx = jnp.arange(C, dtype=jnp.float32)
    rel = idx[:, None] - idx[None, :]
    decay_mask = jnp.where(rel[None] >= 0,
                           jnp.exp(jnp.maximum(rel, 0.0)[None] * log_g[:, None, None]), 0.0)
    q_dec = jnp.exp((idx + 1.0)[None, :] * log_g[:, None])
    k_dec = jnp.exp((C - 1.0 - idx)[None, :] * log_g[:, None])
    chunk_dec = jnp.exp(C * log_g)
    qc = q.reshape(B, N, C, H, d)
    kc = k.reshape(B, N, C, H, d)
    vc = v.reshape(B, N, C, H, d)
    scores = jnp.einsum('bnihd,bnjhd->bnhij', qc, kc).astype(jnp.float32) * decay_mask
    o_inner = jnp.einsum('bnhij,bnjhe->bnihe', scores, vc.astype(jnp.float32))
    kv = jnp.einsum('bnjhd,hj,bnjhe->nbhde', kc.astype(jnp.float32), k_dec,
                    vc.astype(jnp.float32))

    def step(state, kv_n):
        return chunk_dec[None, :, None, None] * state + kv_n, state

    _, s_prev = lax.scan(step, jnp.zeros((B, H, d, d), jnp.float32), kv)
    o_cross = jnp.einsum('bnihd,hi,nbhde->bnihe', qc.astype(jnp.float32), q_dec, s_prev)
    return (o_inner + o_cross).reshape(B, S, H, d)


def causal_depthwise_conv(u, w, b):
    K = w.shape[0]
    S = u.shape[1]
    up = jnp.pad(u, ((0, 0), (K - 1, 0), (0, 0)))
    out = b
    for kk in range(K):
        out = out + up[:, kk:kk + S] * w[kk]
    return out


def rg_lru(xc, w_rg, b_rg, w_ig, b_ig, lam):
    B, S, W = xc.shape
    xb = xc.reshape(B, S, LRU_BLOCKS, LRU_BLOCK_DIM)
    r = jax.nn.sigmoid(jnp.einsum('bsnc,ncd->bsnd', xb, w_rg).reshape(B, S, W) + b_rg)
    i = jax.nn.sigmoid(jnp.einsum('bsnc,ncd->bsnd', xb, w_ig).reshape(B, S, W) + b_ig)
    log_a = (-LRU_C * r * jax.nn.softplus(-lam)).astype(jnp.float32)
    a = jnp.exp(log_a)
    bx = (jnp.sqrt(-jnp.expm1(2.0 * log_a)) * (i * xc)).astype(jnp.float32)

    def combine(left, right):
        a1, b1 = left
        a2, b2 = right
        return a1 * a2, a2 * b1 + b2

    _, h = lax.associative_scan(combine, (a, bx), axis=1)
    return h


def hierarchical_moe(h, w_group, b_group, w_router, b_router, w_gate, w_up, w_down):
    B, S, D = h.shape
    T = B * S
    xt = h.reshape(T, D)
    g_prob = jax.nn.softmax((xt @ w_group + b_group).astype(jnp.float32), axis=-1)
    g_p, g_idx = lax.top_k(g_prob, 1)
    e_logits = (xt @ w_router + b_router).astype(jnp.float32).reshape(T, N_GROUPS, EXPERTS_PER_GROUP)
    e_logits = jnp.take_along_axis(e_logits, g_idx[:, :, None], axis=1)[:, 0]
    e_top, e_loc = lax.top_k(e_logits, TOP_K)
    e_w = jax.nn.softmax(e_top, axis=-1) * g_p
    e_id = g_idx * EXPERTS_PER_GROUP + e_loc

    A = T * TOP_K
    flat_e = e_id.reshape(A)
    flat_w = e_w.reshape(A)
    order = jnp.argsort(flat_e)
    sorted_e = flat_e[order]
    counts = jnp.bincount(flat_e, length=N_EXPERTS)
    padded = (counts + MOE_BLOCK - 1) // MOE_BLOCK * MOE_BLOCK
    start = jnp.cumsum(counts) - counts
    pad_end = jnp.cumsum(padded)
    pad_start = pad_end - padded
    dest = pad_start[sorted_e] + (jnp.arange(A) - start[sorted_e])
    n_blocks = (A + N_EXPERTS * (MOE_BLOCK - 1) + MOE_BLOCK - 1) // MOE_BLOCK
    P = n_blocks * MOE_BLOCK
    tok = jnp.full((P,), T, jnp.int32).at[dest].set((order // TOP_K).astype(jnp.int32))
    wbuf = jnp.zeros((P,), jnp.float32).at[dest].set(flat_w[order])
    block_e = jnp.minimum(
        jnp.searchsorted(pad_end, jnp.arange(n_blocks) * MOE_BLOCK, side='right'), N_EXPERTS - 1)

    x_pad = jnp.concatenate([xt, jnp.zeros((1, D), xt.dtype)], axis=0)
    xs = x_pad[tok].reshape(n_blocks, MOE_BLOCK, D)

    def expert_block(args):
        xb, e = args
        hb = jax.nn.silu(xb @ w_gate[e]) * (xb @ w_up[e])
        return hb @ w_down[e]

    yb = lax.map(expert_block, (xs, block_e))
    y = yb.reshape(P, D) * wbuf[:, None].astype(yb.dtype)
    out = jnp.zeros((T + 1, D), yb.dtype).at[tok].add(y)[:T]
    return out.reshape(B, S, D)


def setup_inputs(seed: int = 0) -> dict:
    key = jax.random.key(seed)
    ks = jax.random.split(key, 24)
    f32 = jnp.float32
    L, D = DEPTH, D_MODEL

    def nrm(k, shape, scale):
        return jax.random.normal(k, shape, f32) * scale

    x = jax.random.normal(ks[0], (BATCH, SEQ, D), f32)
    norm_mix_g = 1.0 + nrm(ks[1], (L, D), 0.02)
    w_in = nrm(ks[2], (L, D, PROJ_IN), D ** -0.5)
    ret_norm_g = 1.0 + nrm(ks[3], (L, RET_WIDTH), 0.02)
    conv_w = nrm(ks[4], (L, CONV_WIDTH, LRU_WIDTH), CONV_WIDTH ** -0.5)
    conv_b = nrm(ks[5], (L, LRU_WIDTH), 0.02)
    w_rg = nrm(ks[6], (L, LRU_BLOCKS, LRU_BLOCK_DIM, LRU_BLOCK_DIM), LRU_BLOCK_DIM ** -0.5)
    b_rg = nrm(ks[7], (L, LRU_WIDTH), 0.02)
    w_ig = nrm(ks[8], (L, LRU_BLOCKS, LRU_BLOCK_DIM, LRU_BLOCK_DIM), LRU_BLOCK_DIM ** -0.5)
    b_ig = nrm(ks[9], (L, LRU_WIDTH), 0.02)
    u = jax.random.uniform(ks[10], (L, LRU_WIDTH), f32, 0.9, 0.999)
    a0 = u ** (1.0 / LRU_C)
    lru_lambda = jnp.log(a0) - jnp.log1p(-a0)
    lru_norm_g = 1.0 + nrm(ks[11], (L, LRU_WIDTH), 0.02)
    w_out = nrm(ks[12], (L, D_MIX, D), D_MIX ** -0.5)
    norm_ffn_g = 1.0 + nrm(ks[13], (L, D), 0.02)
    w_group = nrm(ks[14], (L, D, N_GROUPS), D ** -0.5)
    b_group = nrm(ks[15], (L, N_GROUPS), 0.01)
    w_router = nrm(ks[16], (L, D, N_EXPERTS), D ** -0.5)
    b_router = nrm(ks[17], (L, N_EXPERTS), 0.01)
    w_gate = nrm(ks[18], (L, N_EXPERTS, D, D_EXPERT), D ** -0.5)
    w_up = nrm(ks[19], (L, N_EXPERTS, D, D_EXPERT), D ** -0.5)
    w_down = nrm(ks[20], (L, N_EXPERTS, D_EXPERT, D), D_EXPERT ** -0.5)
    norm_final_g = 1.0 + nrm(ks[21], (D,), 0.02)
    return {"x": x, "norm_mix_g": norm_mix_g, "w_in": w_in, "ret_norm_g": ret_norm_g,
            "conv_w": conv_w, "conv_b": conv_b, "w_rg": w_rg, "b_rg": b_rg,
            "w_ig": w_ig, "b_ig": b_ig, "lru_lambda": lru_lambda, "lru_norm_g": lru_norm_g,
            "w_out": w_out, "norm_ffn_g": norm_ffn_g, "w_group": w_group, "b_group": b_group,
            "w_router": w_router, "b_router": b_router, "w_gate": w_gate, "w_up": w_up,
            "w_down": w_down, "norm_final_g": norm_final_g}


def reference(x, norm_mix_g, w_in, ret_norm_g, conv_w, conv_b, w_rg, b_rg, w_ig, b_ig,
              lru_lambda, lru_norm_g, w_out, norm_ffn_g, w_group, b_group, w_router, b_router,
              w_gate, w_up, w_down, norm_final_g):
    B, S, D = x.shape
    pos = jnp.arange(S)
    R = RET_WIDTH
    splits = [R, 2 * R, 3 * R, 4 * R, 4 * R + LRU_WIDTH]
    for l in range(DEPTH):
        h = rms_norm(x, norm_mix_g[l])
        proj = h @ w_in[l]
        q, k, v, g_ret, u, z = jnp.split(proj, splits, axis=-1)
        q = rope(q.reshape(B, S, RET_HEADS, RET_HEAD_DIM), pos) * (RET_HEAD_DIM ** -0.5)
        k = rope(k.reshape(B, S, RET_HEADS, RET_HEAD_DIM), pos)
        v = v.reshape(B, S, RET_HEADS, RET_HEAD_DIM)
        o = retention_chunkwise(q, k, v)
        mu = jnp.mean(o, axis=-1, keepdims=True)
        var = jnp.mean(jnp.square(o - mu), axis=-1, keepdims=True)
        o = (o - mu) * lax.rsqrt(var + EPS)
        o = o.reshape(B, S, R) * ret_norm_g[l].astype(jnp.float32)
        ret_out = (jax.nn.silu(g_ret.astype(jnp.float32)) * o).astype(x.dtype)
        uc = causal_depthwise_conv(u, conv_w[l], conv_b[l])
        hl = rg_lru(uc, w_rg[l], b_rg[l], w_ig[l], b_ig[l], lru_lambda[l])
        hl = hl.reshape(B, S, LRU_BLOCKS, LRU_BLOCK_DIM)
        hl = hl * lax.rsqrt(jnp.mean(hl * hl, axis=-1, keepdims=True) + EPS)
        hl = hl.reshape(B, S, LRU_WIDTH) * lru_norm_g[l].astype(jnp.float32)
        lru_out = (hl * jax.nn.gelu(z.astype(jnp.float32))).astype(x.dtype)
        mix = jnp.concatenate([ret_out, lru_out], axis=-1)
        x = x + mix @ w_out[l]
        h = rms_norm(x, norm_ffn_g[l])
        x = x + hierarchical_moe(h, w_group[l], b_group[l], w_router[l], b_router[l],
                                 w_gate[l], w_up[l], w_down[l])
    return rms_norm(x, norm_final_g)
```

```python
from contextlib import ExitStack
import math
import numpy as np
import concourse.bass as bass
import concourse.mybir as mybir
from concourse.bass_utils import run_bass_kernel_spmd

F32 = mybir.dt.float32
BF16 = mybir.dt.bfloat16
I32 = mybir.dt.int32
U32 = mybir.dt.uint32
AF = mybir.ActivationFunctionType
ALU = mybir.AluOpType
AX = mybir.AxisListType

NCORES = 8
D = 2048
S = 2048
TPC = 4096
NE = 32
CAP = 384
NS = NE * CAP
EPS = 1e-6
STRIPW = 384 + 2048
GK = 1.5957691216057308


class Buf:
    __slots__ = ("name", "last_w", "readers", "sem", "semcnt")

    def __init__(self, name):
        self.name = name
        self.last_w = None
        self.readers = {}
        self.sem = None
        self.semcnt = 0


class Sched:
    def __init__(self, nc, stack):
        self.nc = nc
        self.stack = stack
        self.engs = {"pe": nc.tensor, "act": nc.scalar, "dve": nc.vector, "pool": nc.gpsimd, "sp": nc.sync}
        self.esem = {k: stack.enter_context(nc.semaphore("e_" + k)) for k in self.engs}
        self.ecnt = {k: 0 for k in self.engs}
        self.known = {k: {} for k in self.engs}
        self.dsems = []
        self.nsem = len(self.engs)

    def _wait(self, eng, ev, skip_sem=None):
        if ev is None:
            return
        sem, val = ev
        if skip_sem is not None and sem is skip_sem:
            return
        kn = self.known[eng]
        if kn.get(id(sem), 0) >= val:
            return
        self.engs[eng].wait_ge(sem, val)
        kn[id(sem)] = val

    def _deps(self, eng, reads, writes, fifo):
        skip = self.esem[eng] if fifo else None
        for b in reads:
            self._wait(eng, b.last_w, skip)
        for b in writes:
            self._wait(eng, b.last_w, skip)
            for sem_id, ev in list(b.readers.items()):
                self._wait(eng, ev, skip)

    def _book(self, ev, reads, writes):
        for b in reads:
            b.readers[id(ev[0])] = ev
        for b in writes:
            b.last_w = ev
            b.readers = {}

    def op(self, eng, fn, reads=(), writes=(), fifo=False):
        self._deps(eng, reads, writes, fifo)
        inst = fn()
        self.ecnt[eng] += 1
        ev = (self.esem[eng], self.ecnt[eng])
        inst.then_inc(ev[0], 1)
        self._book(ev, reads, writes)
        return ev

    def dma(self, eng, fn, reads, writes, dst):
        self._deps(eng, reads, writes, False)
        if dst.sem is None:
            dst.sem = self.stack.enter_context(self.nc.semaphore(f"d{self.nsem}_" + dst.name))
            self.dsems.append(dst)
            self.nsem += 1
        insts = fn()
        if not isinstance(insts, (list, tuple)):
            insts = [insts]
        for inst in insts:
            inst.then_inc(dst.sem, 16)
            dst.semcnt += 16
        ev = (dst.sem, dst.semcnt)
        self._book(ev, reads, writes)
        return ev

    def barrier(self, engines=None):
        for e in (engines or self.engs):
            for o in self.engs:
                if o != e and self.ecnt[o] > 0:
                    self._wait(e, (self.esem[o], self.ecnt[o]))
            for b in self.dsems:
                self._wait(e, (b.sem, b.semcnt))


def build_program(debug=None):
    nc = bass.Bass("TRN2", target_bir_lowering=False)
    T, V, A, G, SP = nc.tensor, nc.vector, nc.scalar, nc.gpsimd, nc.sync

    def din(name, shape, dt=F32):
        return nc.dram_tensor(name, shape, dt, kind="ExternalInput").ap()

    x = din("x", [TPC, D])
    w_in = din("w_in", [48, 128, 16 * 128])
    w_out = din("w_out", [D, D])
    pvec = din("pvec", [128, 128])
    wgates = din("wgates", [16, 128, 128])
    wr = din("wr", [D, 36])
    brow = din("brow", [128, 36])
    gfin = din("gfin", [128, D])
    gffn = din("gffn", [128, D])
    cst = din("cst", [2, 128, S])
    dmask = din("dmask", [8, 128, 4 * 512])
    cfq = din("cfq", [8, 128, 512])
    ctab = din("ctab", [128, 128])
    w_gate = din("w_gate", [NE, D, 1024])
    w_up = din("w_up", [NE, D, 1024])
    w_down = din("w_down", [NE, 1024, D])
    out = nc.dram_tensor("out", [TPC, D], F32, kind="ExternalOutput").ap()

    def dscr(name, shape, dt):
        return nc.dram_tensor(name, shape, dt, kind="Internal").ap()

    mixT_scr = dscr("mixT_scr", [16, 128, S], BF16)
    xmid_scr = dscr("xmid_scr", [TPC, D], F32)
    h2_scr = dscr("h2_scr", [TPC + 128, D], BF16)
    slot_tab = dscr("slot_tab", [NS + 128, 2], F32)
    yslot_scr = dscr("yslot_scr", [NS + 128, D], F32)

    dbg = {}
    if debug == "mix":
        dbg["mix"] = nc.dram_tensor("dbg_mix", [16, 128, S], BF16, kind="ExternalOutput").ap()
    if debug == "xmid":
        dbg["xmid"] = nc.dram_tensor("dbg_xmid", [TPC, D], F32, kind="ExternalOutput").ap()
        dbg["h2"] = nc.dram_tensor("dbg_h2", [TPC + 128, D], BF16, kind="ExternalOutput").ap()
        dbg["stab"] = nc.dram_tensor("dbg_stab", [NS + 128, 2], F32, kind="ExternalOutput").ap()
        dbg["gix"] = nc.dram_tensor("dbg_gix", [128, 64], I32, kind="ExternalOutput").ap()
        dbg["lg"] = nc.dram_tensor("dbg_lg", [128, 24 * 40], F32, kind="ExternalOutput").ap()
    if debug == "moe":
        dbg["ys"] = nc.dram_tensor("dbg_ys", [NS + 128, D], F32, kind="ExternalOutput").ap()
    stop = [False]
    PV_GMIX, PV_GFFN, PV_GRET, PV_CW, PV_CB, PV_BRG, PV_BIG, PV_LAM, PV_GLRU = 0, 16, 32, 40, 72, 80, 88, 96, 104

    with ExitStack() as top:
        Sx = Sched(nc, top)
        uniq = [0]

        def sb(name, shape, dt, st=top):
            uniq[0] += 1
            return st.enter_context(nc.sbuf_tensor(f"{name}_{uniq[0]}", shape, dt))

        NPS = 6
        psf = [top.enter_context(nc.psum_tensor(f"psf{i}", [128, 512], F32)) for i in range(NPS)]
        psf_b = [Buf(f"psf{i}") for i in range(NPS)]
        gen_ring = [0, 1, 2, 3]
        ps_rr = [0]

        acc_rr = [0]

        def bank():
            i = gen_ring[ps_rr[0] % len(gen_ring)]
            ps_rr[0] += 1
            return psf[i], psf_b[i]

        def accbank():
            i = 4 + acc_rr[0] % 2
            acc_rr[0] += 1
            return psf[i], psf_b[i]

        pv = sb("pv", [128, 128], F32); pv_b = Buf("pv")
        identf = sb("identf", [128, 128], F32); identf_b = Buf("identf")
        identb = sb("identb", [128, 128], BF16); identb_b = Buf("identb")
        onesb = sb("onesb", [128, 128], BF16); onesb_b = Buf("onesb")
        onefull = sb("onefull", [128, 128], BF16); onefull_b = Buf("onefull")
        ustr = sb("ustr", [128, 128], BF16); ustr_b = Buf("ustr")
        iota_e = sb("iota_e", [128, 32], F32); iota_e_b = Buf("iota_e")
        dummyp = sb("dummyp", [128, 1], F32); dummyp_b = Buf("dummyp")
        tokid = sb("tokid", [128, 32], F32); tokid_b = Buf("tokid")
        gix = sb("gix", [128, 32, 2], I32); gix_b = Buf("gix")
        Rb = sb("Rb", [128, 32], BF16); Rb_b = Buf("Rb")
        wrs = sb("wrs", [128, 16, 36], F32); wrs_b = Buf("wrs")
        brs = sb("brs", [128, 36], F32); brs_b = Buf("brs")
        wg_sb = sb("wg_sb", [128, 16, 128], BF16); wg_b = Buf("wg_sb")
        scl = sb("scl", [128, 8], F32); scl_b = Buf("scl")
        tmpi = sb("tmpi", [128, 128], I32); tmpi_b = Buf("tmpi")
        tmpf = sb("tmpf", [128, 128], F32); tmpf_b = Buf("tmpf")

        Sx.dma("sp", lambda: SP.dma_start(out=pv[:], in_=pvec), [], [pv_b], pv_b)
        Sx.dma("sp", lambda: SP.dma_start(out=wrs[:], in_=wr.rearrange("(kc p) n -> p kc n", p=128)), [], [wrs_b], wrs_b)
        Sx.dma("sp", lambda: SP.dma_start(out=brs[:], in_=brow), [], [brs_b], brs_b)
        ctab_sb = sb("ctab_sb", [128, 128], F32); ctab_b = Buf("ctab_sb")
        Sx.dma("sp", lambda: SP.dma_start(out=ctab_sb[:], in_=ctab), [], [ctab_b], ctab_b)
        Sx.dma("pool", lambda: G.dma_start(out=wg_sb[:], in_=wgates.rearrange("n c d -> c n d")), [], [wg_b], wg_b)
        Sx.op("pool", lambda: G.iota(tmpi[:], pattern=[[1, 128]], base=0, channel_multiplier=-1), [], [tmpi_b])
        Sx.op("dve", lambda: V.tensor_copy(out=tmpf[:], in_=tmpi[:]), [tmpi_b], [tmpf_b])
        Sx.op("dve", lambda: V.tensor_single_scalar(out=identf[:], in_=tmpf[:], scalar=0.0, op=ALU.is_equal), [tmpf_b], [identf_b])
        Sx.op("dve", lambda: V.tensor_copy(out=identb[:], in_=identf[:]), [identf_b], [identb_b])
        Sx.op("dve", lambda: V.tensor_single_scalar(out=ustr[:], in_=tmpf[:], scalar=0.0, op=ALU.is_gt), [tmpf_b], [ustr_b])
        permf = sb("permf", [128, 128], F32); permf_b = Buf("permf")
        Sx.op("dve", lambda: V.tensor_single_scalar(out=permf[:], in_=tmpf[:], scalar=64.0, op=ALU.is_equal), [tmpf_b], [permf_b])
        Sx.op("dve", lambda: V.tensor_single_scalar(out=tmpf[:, :], in_=tmpf[:], scalar=-64.0, op=ALU.is_equal), [tmpf_b], [tmpf_b])
        Sx.op("dve", lambda: V.tensor_tensor(out=permf[:], in0=permf[:], in1=tmpf[:], op=ALU.add), [permf_b, tmpf_b], [permf_b])
        Sx.op("dve", lambda: V.memset(onesb[:], 1.0 / 128.0), [], [onesb_b])
        Sx.op("dve", lambda: V.memset(onefull[:], 1.0), [], [onefull_b])
        Sx.op("dve", lambda: V.memset(Rb[:], 0.0), [], [Rb_b])
        Sx.op("pool", lambda: G.iota(tmpi[:, 0:32], pattern=[[1, 32]], base=0, channel_multiplier=0), [tmpf_b], [tmpi_b])
        Sx.op("dve", lambda: V.tensor_copy(out=iota_e[:], in_=tmpi[:, 0:32]), [tmpi_b], [iota_e_b])
        Sx.op("pool", lambda: G.iota(tmpi[:, 0:32], pattern=[[128, 32]], base=0, channel_multiplier=1), [iota_e_b], [tmpi_b])
        Sx.op("dve", lambda: V.tensor_copy(out=tokid[:], in_=tmpi[:, 0:32]), [tmpi_b], [tokid_b])
        Sx.op("pool", lambda: G.iota(tmpi[:, 0:1], pattern=[[0, 1]], base=NS, channel_multiplier=1), [tokid_b], [tmpi_b])
        Sx.op("dve", lambda: V.tensor_copy(out=dummyp[:], in_=tmpi[:, 0:1]), [tmpi_b], [dummyp_b])
        Sx.op("act", lambda: A.activation(out=scl[:], in_=pv[:, PV_LAM:PV_LAM + 8], func=AF.Exp, scale=-1.0), [pv_b], [scl_b])
        Sx.op("act", lambda: A.activation(out=scl[:], in_=scl[:], func=AF.Ln, bias=1.0), [scl_b], [scl_b])
        Sx.op("dve", lambda: V.tensor_scalar(out=scl[:], in0=scl[:], scalar1=-8.0, scalar2=None, op0=ALU.mult), [scl_b], [scl_b])

        with ExitStack() as ini:
            zt = sb("zt", [128, D], F32, ini); zt_b = Buf("zt")
            zb = sb("zb", [128, D], BF16, ini); zb_b = Buf("zb")
            sti = sb("sti", [128, NS // 128, 2], F32, ini); sti_b = Buf("sti")
            h2z_b, ysz_b, stab_b = Buf("h2z"), Buf("ysz"), Buf("stab")
            Sx.op("dve", lambda: V.memset(zt[:], 0.0), [], [zt_b])
            Sx.op("dve", lambda: V.memset(zb[:], 0.0), [], [zb_b])
            Sx.op("dve", lambda: V.memset(sti[:, :, 0:1], float(TPC)), [], [sti_b])
            Sx.op("dve", lambda: V.memset(sti[:, :, 1:2], 0.0), [sti_b], [sti_b])
            Sx.dma("sp", lambda: SP.dma_start(out=h2_scr[TPC:TPC + 128, :], in_=zb[:]), [zb_b], [h2z_b], h2z_b)
            Sx.dma("sp", lambda: SP.dma_start(out=yslot_scr[NS:NS + 128, :], in_=zt[:]), [zt_b], [ysz_b], ysz_b)
            Sx.dma("sp", lambda: SP.dma_start(out=slot_tab[0:NS, :].rearrange("(p r) c -> p r c", p=128), in_=sti[:]),
                   [sti_b], [stab_b], stab_b)
            Sx.barrier()

        xmid_b = [Buf(f"xmid_scr{i}") for i in range(2)]
        h2s_b = [Buf(f"h2_scr{i}") for i in range(2)]
        mixs_b = [Buf(f"mixT_scr{i}") for i in range(4)]
        ysl_b = [Buf(f"yslot_scr{i}") for i in range(2)]

        def rstd_from_ss(ss, ss_b, n_over):
            Sx.op("act", lambda: A.activation(out=ss, in_=ss, func=AF.Ln, scale=1.0 / n_over, bias=EPS), [ss_b], [ss_b])
            Sx.op("act", lambda: A.activation(out=ss, in_=ss, func=AF.Exp, scale=-0.5), [ss_b], [ss_b])

        def mm_group(outp, out_b, pairs, reads):
            def fn():
                last = None
                n = len(pairs)
                for i, (l, r) in enumerate(pairs):
                    last = T.matmul(outp, lhsT=l, rhs=r, start=(i == 0), stop=(i == n - 1))
                return last
            return Sx.op("pe", fn, reads, [out_b])

        for s in range(2):
            with ExitStack() as r1:
                R1 = sb("R1", [128, 16 * 2048], BF16, r1)
                hT = R1[:].rearrange("p (k t) -> p k t", k=16)
                hT_b = [[Buf(f"hT{t}_{k}") for k in range(16)] for t in range(16)]
                with ExitStack() as a1:
                    xin = [sb(f"xin{i}", [128, D], F32, a1) for i in range(2)]
                    xin_b = [Buf(f"xin{i}") for i in range(2)]
                    junk = sb("junk", [128, D], BF16, a1); junk_b = Buf("junk")
                    ssa = [sb(f"ssa{i}", [128, 1], F32, a1) for i in range(2)]
                    ssa_b = [Buf(f"ssa{i}") for i in range(2)]
                    def a1_stats(t):
                        sl = t % 2
                        r0 = s * S + t * 128
                        Sx.dma("sp", lambda: SP.dma_start(out=xin[sl][:], in_=x[r0:r0 + 128, :]), [], [xin_b[sl]], xin_b[sl])
                        Sx.op("act", lambda: A.activation(out=junk[:], in_=xin[sl][:], func=AF.Square, accum_out=ssa[sl][:]),
                              [xin_b[sl]], [junk_b, ssa_b[sl]])
                        rstd_from_ss(ssa[sl][:], ssa_b[sl], D)
                        Sx.op("dve", lambda: V.tensor_scalar(out=xin[sl][:], in0=xin[sl][:], scalar1=ssa[sl][:, 0:1], scalar2=None, op0=ALU.mult),
                              [xin_b[sl], ssa_b[sl]], [xin_b[sl]])

                    def a1_tr(t):
                        sl = t % 2
                        for q in range(4):
                            pt, pb = bank()
                            def fn():
                                last = None
                                for j in range(4):
                                    kc = q * 4 + j
                                    last = T.transpose(out=pt[:, j * 128:(j + 1) * 128], in_=xin[sl][:, kc * 128:(kc + 1) * 128], identity=identf[:])
                                return last
                            Sx.op("pe", fn, [xin_b[sl], identf_b], [pb])
                            for j in range(4):
                                kc = q * 4 + j
                                if q % 2 == 0:
                                    Sx.op("dve", lambda: V.tensor_scalar(out=hT[:, kc, t * 128:(t + 1) * 128], in0=pt[:, j * 128:(j + 1) * 128],
                                                                         scalar1=pv[:, PV_GMIX + kc:PV_GMIX + kc + 1], scalar2=None, op0=ALU.mult),
                                          [pb, pv_b], [hT_b[t][kc]])
                                else:
                                    Sx.op("act", lambda: A.activation(out=hT[:, kc, t * 128:(t + 1) * 128], in_=pt[:, j * 128:(j + 1) * 128],
                                                                      func=AF.Copy, scale=pv[:, PV_GMIX + kc:PV_GMIX + kc + 1]),
                                          [pb, pv_b], [hT_b[t][kc]])

                    a1_stats(0)
                    for t in range(16):
                        if t + 1 < 16:
                            a1_stats(t + 1)
                        a1_tr(t)
                    Sx.barrier()

                with ExitStack() as a2:
                    for i_ in range(2):
                        psf.append(a2.enter_context(nc.psum_tensor(f"psx{s}_{i_}", [128, 512], F32)))
                        psf_b.append(Buf(f"psx{i_}"))
                    gen_ring[:] = [0, 1, 2, 3, 6, 7]
                    ps_rr[0] = 0
                    NW = 8
                    wring = [sb(f"wring{i}", [128, 16, 128], BF16, a2) for i in range(NW)]
                    wring_b = [Buf(f"wring{i}") for i in range(NW)]
                    wr_rr = [0]

                    def load_chunk(c):
                        i = wr_rr[0] % NW
                        wr_rr[0] += 1
                        Sx.dma("pool", lambda: G.dma_start(out=wring[i][:].rearrange("p k j -> p (k j)").rearrange("p (a n) -> p a n", a=2),
                                                           in_=w_in[c].rearrange("p (a n) -> p a n", a=2)),
                               [], [wring_b[i]], wring_b[i])
                        return wring[i], wring_b[i]

                    def proj_fm(wt, wt_b, tg):
                        pt, pb = bank()
                        mm_group(pt[:], pb, [(wt[:, kc, :], hT[:, kc, tg * 512:(tg + 1) * 512]) for kc in range(16)],
                                 [wt_b] + [b_ for tt_ in range(tg * 4, tg * 4 + 4) for b_ in hT_b[tt_]])
                        return pt, pb

                    with ExitStack() as ar:
                        cs = sb("cs", [128, 2, S], F32, ar); cs_b = Buf("cs")
                        Sx.dma("sp", lambda: SP.dma_start(out=cs[:], in_=cst.rearrange("c p t -> p c t")), [], [cs_b], cs_b)
                        qk = [sb(f"qk{i}", [128, S], BF16, ar) for i in range(4)]
                        qk_b = [Buf(f"qk{i}") for i in range(4)]
                        rt = [sb(f"rt{i}", [128, 512], F32, ar) for i in range(4)]
                        rt_b = [Buf(f"rt{i}") for i in range(4)]
                        v_sb = sb("v_sb", [128, 16, 256], BF16, ar); v_b = Buf("v_sb")
                        gs = [sb(f"gs{i}", [128, S], BF16, ar) for i in range(2)]
                        gs_b = [Buf(f"gs{i}") for i in range(2)]
                        strip = sb("dmk", [128, 4, 512], F32, ar); strip_b = Buf("dmk")
                        cfp = sb("cfp", [128, 2, 512], F32, ar); cfp_b = Buf("cfp")
                        NST = 4
                        sT = [sb(f"sT{i}", [128, 512], BF16, ar) for i in range(NST)]
                        sT_b = [Buf(f"sT{i}") for i in range(NST)]
                        st_rr = [0]
                        ob = sb("ob", [128, 512], BF16, ar); ob_b = Buf("ob")
                        osq = sb("osq", [128, 512], BF16, ar); osq_b = Buf("osq")
                        mean_sb = sb("mean_sb", [128, 512], F32, ar); mean_b = Buf("mean_sb")
                        var_sb = sb("var_sb", [128, 512], F32, ar); var_b = Buf("var_sb")
                        cen = sb("cen", [128, 512], F32, ar); cen_b = Buf("cen")
                        mixrow = sb("mixrow", [128, 2, S], BF16, ar); mixrow_b = Buf("mixrow")

                        pend = [None]

                        def norm_steps(hh, h, qs, po_t, po_b):
                            Sx.op("act", lambda: A.activation(out=ob[:], in_=po_t[:], func=AF.Copy), [po_b], [ob_b])
                            Sx.op("act", lambda: A.activation(out=osq[:], in_=po_t[:], func=AF.Square), [po_b], [osq_b])
                            yield
                            pm, pm_b = bank()
                            mm_group(pm[:], pm_b, [(onesb[:], ob[:])], [onesb_b, ob_b])
                            pq, pq_b = bank()
                            mm_group(pq[:], pq_b, [(onesb[:], osq[:])], [onesb_b, osq_b])
                            Sx.op("act", lambda: A.activation(out=mean_sb[:], in_=pm[:], func=AF.Copy), [pm_b], [mean_b])
                            Sx.op("dve", lambda: V.tensor_tensor(out=var_sb[:], in0=mean_sb[:], in1=mean_sb[:], op=ALU.mult), [mean_b], [var_b])
                            Sx.op("dve", lambda: V.tensor_tensor(out=var_sb[:], in0=pq[:], in1=var_sb[:], op=ALU.subtract), [pq_b, var_b], [var_b])
                            yield
                            Sx.op("dve", lambda: V.tensor_scalar(out=var_sb[:], in0=var_sb[:], scalar1=0.0, scalar2=None, op0=ALU.max), [var_b], [var_b])
                            Sx.op("act", lambda: A.activation(out=var_sb[:], in_=var_sb[:], func=AF.Ln, bias=EPS), [var_b], [var_b])
                            Sx.op("act", lambda: A.activation(out=var_sb[:], in_=var_sb[:], func=AF.Exp, scale=-0.5), [var_b], [var_b])
                            yield
                            Sx.op("dve", lambda: V.tensor_tensor(out=cen[:], in0=po_t[:], in1=mean_sb[:], op=ALU.subtract), [po_b, mean_b], [cen_b])
                            Sx.op("dve", lambda: V.tensor_tensor(out=cen[:], in0=cen[:], in1=var_sb[:], op=ALU.mult), [cen_b, var_b], [cen_b])
                            Sx.op("dve", lambda: V.scalar_tensor_tensor(out=mixrow[:, hh, qs], in0=cen[:], scalar=pv[:, PV_GRET + h:PV_GRET + h + 1],
                                                                        in1=gs[hh][:, qs], op0=ALU.mult, op1=ALU.mult),
                                  [cen_b, pv_b, gs_b[hh]], [mixrow_b])

                        for p in range(4):
                            c0 = p * 8
                            Sx.dma("sp", lambda: SP.dma_start(out=cfp[:], in_=cfq[2 * p:2 * p + 2].rearrange("h p t -> p h t")), [], [cfp_b], cfp_b)
                            for qi in range(2):
                                for hh in range(2):
                                    wa, wa_b = load_chunk(c0 + 2 * qi + hh)
                                    dst, dst_b = qk[2 * qi + hh], qk_b[2 * qi + hh]
                                    for tg in range(4):
                                        pa, pa_b = proj_fm(wa, wa_b, tg)
                                        ts = slice(tg * 512, (tg + 1) * 512)
                                        ri = (tg % 2) * 2
                                        Sx.op("act", lambda: A.activation(out=rt[ri][:], in_=pa[:], func=AF.Copy), [pa_b], [rt_b[ri]])
                                        pp_, pp_b_ = bank()
                                        mm_group(pp_[:], pp_b_, [(permf[:], rt[ri][:])], [permf_b, rt_b[ri]])
                                        Sx.op("dve", lambda: V.tensor_tensor(out=rt[ri + 1][:], in0=pp_[:], in1=cs[:, 1, ts], op=ALU.mult), [pp_b_, cs_b], [rt_b[ri + 1]])
                                        Sx.op("dve", lambda: V.tensor_tensor(out=rt[ri][:], in0=rt[ri][:], in1=cs[:, 0, ts], op=ALU.mult), [rt_b[ri], cs_b], [rt_b[ri]])
                                        if qi == 0:
                                            Sx.op("dve", lambda: V.tensor_tensor(out=rt[ri][:], in0=rt[ri][:], in1=rt[ri + 1][:], op=ALU.add), [rt_b[ri], rt_b[ri + 1]], [rt_b[ri]])
                                            Sx.op("dve", lambda: V.tensor_tensor(out=dst[:, ts], in0=rt[ri][:], in1=cfp[:, hh, :], op=ALU.mult), [rt_b[ri], cfp_b], [dst_b])
                                        else:
                                            Sx.op("dve", lambda: V.tensor_tensor(out=dst[:, ts], in0=rt[ri][:], in1=rt[ri + 1][:], op=ALU.add), [rt_b[ri], rt_b[ri + 1]], [dst_b])
                            wv = [load_chunk(c0 + 4), load_chunk(c0 + 5)]
                            for t2 in range(8):
                                pt, pb = bank()
                                def fn():
                                    last = None
                                    for tt in range(2):
                                        t = t2 * 2 + tt
                                        for hh in range(2):
                                            for kc in range(16):
                                                last = T.matmul(pt[:, tt * 256 + hh * 128: tt * 256 + hh * 128 + 128],
                                                                lhsT=hT[:, kc, t * 128:(t + 1) * 128], rhs=wv[hh][0][:, kc, :],
                                                                start=(kc == 0), stop=(kc == 15))
                                    return last
                                Sx.op("pe", fn, [wv[0][1], wv[1][1]] + hT_b[2 * t2] + hT_b[2 * t2 + 1], [pb])
                                Sx.op("act", lambda: A.activation(out=v_sb[:, 2 * t2:2 * t2 + 2, :], in_=pt[:].rearrange("p (a b) -> p a b", a=2), func=AF.Copy),
                                      [pb], [v_b])
                            for hh in range(2):
                                wgt, wgt_b = load_chunk(c0 + 6 + hh)
                                for tg in range(4):
                                    pt, pb = proj_fm(wgt, wgt_b, tg)
                                    Sx.op("act", lambda: A.activation(out=gs[hh][:, tg * 512:(tg + 1) * 512], in_=pt[:], func=AF.Silu), [pb], [gs_b[hh]])
                            for hh in range(2):
                                h = 2 * p + hh
                                po = hh * 64
                                Sx.dma("sp", lambda: SP.dma_start(out=strip[:], in_=dmask[h].rearrange("p (r t) -> p r t", r=4)), [], [strip_b], strip_b)
                                for g in range(4):
                                    nJ = 4 * g + 4
                                    qs = slice(g * 512, (g + 1) * 512)
                                    po_t, po_b = accbank()

                                    def scores(J):
                                        pt, pb = bank()
                                        ks = slice(J * 128, (J + 1) * 128)
                                        mm_group(pt[:], pb, [(qk[2 + hh][:, ks], qk[hh][:, qs])], [qk_b[hh], qk_b[2 + hh]])
                                        return pt, pb
                                    ahead = [scores(0)]
                                    if nJ > 1:
                                        ahead.append(scores(1))
                                    for J in range(nJ):
                                        cur = ahead.pop(0)
                                        if J + 2 < nJ:
                                            ahead.append(scores(J + 2))
                                        i = st_rr[0] % NST
                                        st_rr[0] += 1
                                        if J < 4 * g:
                                            cc = h * 16 + (4 * g - J)
                                            Sx.op("act", lambda: A.activation(out=sT[i][:], in_=cur[0][:], func=AF.Copy, scale=ctab_sb[:, cc:cc + 1]),
                                                  [cur[1], ctab_b], [sT_b[i]])
                                        else:
                                            Sx.op("dve", lambda: V.tensor_tensor(out=sT[i][:], in0=cur[0][:], in1=strip[:, J - 4 * g, :], op=ALU.mult),
                                                  [cur[1], strip_b], [sT_b[i]])
                                        Sx.op("pe", lambda: T.matmul(po_t[:], lhsT=v_sb[:, J, hh * 128:(hh + 1) * 128], rhs=sT[i][:],
                                                                     start=(J == 0), stop=(J == nJ - 1)),
                                              [v_b, sT_b[i]], [po_b], fifo=True)
                                        if pend[0] is not None:
                                            if next(pend[0], "done") == "done":
                                                pend[0] = None
                                    while pend[0] is not None:
                                        if next(pend[0], "done") == "done":
                                            pend[0] = None
                                    pend[0] = norm_steps(hh, h, qs, po_t, po_b)
                            while pend[0] is not None:
                                if next(pend[0], "done") == "done":
                                    pend[0] = None
                            mb = mixs_b[p % 4]
                            Sx.dma("sp", lambda: SP.dma_start(out=mixT_scr[2 * p:2 * p + 2].rearrange("c p t -> p c t"), in_=mixrow[:]),
                                   [mixrow_b], [mb], mb)
                        Sx.barrier()

                    with ExitStack() as al:
                        ubuf = sb("ubuf", [128, S + 4], F32, al); ubuf_b = Buf("ubuf")
                        uc = sb("uc", [128, S], F32, al); uc_b = Buf("uc")
                        ucb = sb("ucb", [128, S], BF16, al); ucb_b = Buf("ucb")
                        abuf = sb("abuf", [128, S], F32, al); abuf_b = Buf("abuf")
                        ibuf = sb("ibuf", [128, S], F32, al); ibuf_b = Buf("ibuf")
                        hbuf = sb("hbuf", [128, S], F32, al); hbuf_b = Buf("hbuf")
                        gzb = sb("gzb", [128, S], BF16, al); gzb_b = Buf("gzb")
                        hsq = sb("hsq", [128, S], BF16, al); hsq_b = Buf("hsq")
                        mrl = sb("mrl", [128, S], BF16, al); mrl_b = Buf("mrl")
                        zs = sb("zs", [128, 512], F32, al); zs_b = Buf("zs")
                        z2 = sb("z2", [128, 512], F32, al); z2_b = Buf("z2")
                        rs2 = [sb(f"rs{i}", [128, 512], F32, al) for i in range(2)]
                        rs2_b = [Buf(f"rs{i}") for i in range(2)]
                        Sx.op("dve", lambda: V.memset(ubuf[:, 0:3], 0.0), [], [ubuf_b])
                        gzb2 = [gzb, sb("gzb1", [128, S], BF16, al)]
                        gzb2_b = [gzb_b, Buf("gzb1")]

                        def proj_gen(n):
                            wu, wu_b = load_chunk(32 + 2 * n)
                            wz, wz_b = load_chunk(33 + 2 * n)
                            for tg in range(4):
                                pt, pb = proj_fm(wu, wu_b, tg)
                                Sx.op("act", lambda: A.activation(out=ubuf[:, 3 + tg * 512:3 + (tg + 1) * 512], in_=pt[:], func=AF.Copy), [pb], [ubuf_b])
                                yield
                            gz, gz_b = gzb2[n % 2], gzb2_b[n % 2]
                            for tg in range(4):
                                ts = slice(tg * 512, (tg + 1) * 512)
                                pt, pb = proj_fm(wz, wz_b, tg)
                                Sx.op("act", lambda: A.activation(out=zs[:], in_=pt[:], func=AF.Copy), [pb], [zs_b])
                                Sx.op("act", lambda: A.activation(out=z2[:], in_=zs[:], func=AF.Square), [zs_b], [z2_b])
                                yield
                                Sx.op("dve", lambda: V.tensor_scalar(out=z2[:], in0=z2[:], scalar1=0.044715 * GK, scalar2=GK, op0=ALU.mult, op1=ALU.add), [z2_b], [z2_b])
                                Sx.op("dve", lambda: V.tensor_tensor(out=z2[:], in0=z2[:], in1=zs[:], op=ALU.mult), [z2_b, zs_b], [z2_b])
                                Sx.op("act", lambda: A.activation(out=z2[:], in_=z2[:], func=AF.Sigmoid), [z2_b], [z2_b])
                                Sx.op("dve", lambda: V.tensor_tensor(out=gz[:, ts], in0=z2[:], in1=zs[:], op=ALU.mult), [z2_b, zs_b], [gz_b])
                                yield

                        def chain_gen(n):
                            gz, gz_b = gzb2[n % 2], gzb2_b[n % 2]
                            cw = lambda k: pv[:, PV_CW + n * 4 + k:PV_CW + n * 4 + k + 1]
                            Sx.op("dve", lambda: V.tensor_scalar(out=uc[:], in0=ubuf[:, 0:S], scalar1=cw(0), scalar2=pv[:, PV_CB + n:PV_CB + n + 1],
                                                                 op0=ALU.mult, op1=ALU.add), [ubuf_b, pv_b], [uc_b])
                            for k in range(1, 4):
                                Sx.op("dve", lambda: V.scalar_tensor_tensor(out=uc[:], in0=ubuf[:, k:k + S], scalar=cw(k), in1=uc[:], op0=ALU.mult, op1=ALU.add),
                                      [ubuf_b, pv_b, uc_b], [uc_b])
                            yield "ubuf_free"
                            Sx.op("act", lambda: A.activation(out=ucb[:], in_=uc[:], func=AF.Copy), [uc_b], [ucb_b])
                            yield
                            for tg in range(4):
                                ts = slice(tg * 512, (tg + 1) * 512)
                                pt, pb = bank()
                                mm_group(pt[:], pb, [(wg_sb[:, n, :], ucb[:, ts])], [wg_b, ucb_b])
                                Sx.op("act", lambda: A.activation(out=abuf[:, ts], in_=pt[:], func=AF.Sigmoid, bias=pv[:, PV_BRG + n:PV_BRG + n + 1]), [pb, pv_b], [abuf_b])
                                pt2, pb2 = bank()
                                mm_group(pt2[:], pb2, [(wg_sb[:, 8 + n, :], ucb[:, ts])], [wg_b, ucb_b])
                                Sx.op("act", lambda: A.activation(out=ibuf[:, ts], in_=pt2[:], func=AF.Sigmoid, bias=pv[:, PV_BIG + n:PV_BIG + n + 1]), [pb2, pv_b], [ibuf_b])
                                yield
                            Sx.op("act", lambda: A.activation(out=abuf[:], in_=abuf[:], func=AF.Exp, scale=scl[:, n:n + 1]), [abuf_b, scl_b], [abuf_b])
                            yield
                            Sx.op("dve", lambda: V.tensor_tensor(out=hbuf[:], in0=abuf[:], in1=abuf[:], op=ALU.mult), [abuf_b], [hbuf_b])
                            Sx.op("dve", lambda: V.tensor_scalar(out=hbuf[:], in0=hbuf[:], scalar1=-1.0, scalar2=1.0, op0=ALU.mult, op1=ALU.add), [hbuf_b], [hbuf_b])
                            yield
                            Sx.op("act", lambda: A.activation(out=hbuf[:], in_=hbuf[:], func=AF.Sqrt), [hbuf_b], [hbuf_b])
                            yield
                            Sx.op("dve", lambda: V.tensor_tensor(out=ibuf[:], in0=ibuf[:], in1=hbuf[:], op=ALU.mult), [ibuf_b, hbuf_b], [ibuf_b])
                            yield
                            Sx.op("dve", lambda: V.tensor_tensor(out=ibuf[:], in0=ibuf[:], in1=uc[:], op=ALU.mult), [ibuf_b, uc_b], [ibuf_b])
                            yield
                            Sx.op("dve", lambda: V.tensor_tensor_scan(out=hbuf[:], data0=abuf[:], data1=ibuf[:], initial=0.0, op0=ALU.mult, op1=ALU.add),
                                  [abuf_b, ibuf_b], [hbuf_b])
                            yield
                            Sx.op("act", lambda: A.activation(out=hsq[:], in_=hbuf[:], func=AF.Square), [hbuf_b], [hsq_b])
                            yield
                            for tg in range(4):
                                ts = slice(tg * 512, (tg + 1) * 512)
                                pt, pb = bank()
                                rs, rs_b = rs2[tg % 2], rs2_b[tg % 2]
                                mm_group(pt[:], pb, [(onesb[:], hsq[:, ts])], [onesb_b, hsq_b])
                                Sx.op("act", lambda: A.activation(out=rs[:], in_=pt[:], func=AF.Ln, bias=EPS), [pb], [rs_b])
                                Sx.op("act", lambda: A.activation(out=rs[:], in_=rs[:], func=AF.Exp, scale=-0.5), [rs_b], [rs_b])
                                Sx.op("dve", lambda: V.tensor_tensor(out=rs[:], in0=rs[:], in1=hbuf[:, ts], op=ALU.mult), [rs_b, hbuf_b], [rs_b])
                                Sx.op("dve", lambda: V.scalar_tensor_tensor(out=mrl[:, ts], in0=rs[:], scalar=pv[:, PV_GLRU + n:PV_GLRU + n + 1], in1=gz[:, ts],
                                                                            op0=ALU.mult, op1=ALU.mult), [rs_b, pv_b, gz_b], [mrl_b])
                                yield
                            mb = mixs_b[n % 4]
                            Sx.dma("sp", lambda: SP.dma_start(out=mixT_scr[8 + n], in_=mrl[:]), [mrl_b], [mb], mb)

                        pg0 = proj_gen(0)
                        for _ in pg0:
                            pass
                        for n in range(8):
                            cg = chain_gen(n)
                            while next(cg, "done") != "ubuf_free":
                                pass
                            pg = proj_gen(n + 1) if n + 1 < 8 else None
                            cdone, pdone = False, pg is None
                            while not (cdone and pdone):
                                if not cdone and next(cg, "done") == "done":
                                    cdone = True
                                if not pdone and next(pg, "done") == "done":
                                    pdone = True
                        Sx.barrier()

            del psf[6:], psf_b[6:]
            gen_ring[:] = [0, 1, 2, 3]
            ps_rr[0] = 0
            if debug == "mix" and s == 0:
                db_ = Buf("dbgmix")
                Sx.dma("sp", lambda: SP.dma_start(out=dbg["mix"], in_=mixT_scr), mixs_b, [db_], db_)
                Sx.barrier()
                stop[0] = True
                break
            with ExitStack() as pb_:
                wo = sb("wo", [128, 16, D], BF16, pb_); wo_b = [Buf(f"wo{i}") for i in range(4)]
                wov = w_out.rearrange("(kc p) n -> p kc n", p=128)
                for i in range(4):
                    Sx.dma("pool", lambda: [G.dma_start(out=wo[:, kc, :].rearrange("p (a n) -> p a n", a=2), in_=wov[:, kc, :].rearrange("p (a n) -> p a n", a=2))
                                            for kc in range(4 * i, 4 * i + 4)], [], [wo_b[i]], wo_b[i])
                mixg = [sb(f"mixg{i}", [128, 16, 512], BF16, pb_) for i in range(2)]
                mixg_b = [Buf(f"mixg{i}") for i in range(2)]
                xin = [sb(f"xinb{i}", [128, D], F32, pb_) for i in range(2)]
                xin_b = [Buf(f"xinb{i}") for i in range(2)]
                xm = [sb(f"xm{i}", [128, D], F32, pb_) for i in range(2)]
                xm_b = [Buf(f"xm{i}") for i in range(2)]
                h2b = [sb(f"h2b{i}", [128, D], BF16, pb_) for i in range(2)]
                h2b_b = [Buf(f"h2b{i}") for i in range(2)]
                gfb = sb("gfb", [128, D], F32, pb_); gfb_b = Buf("gfb")
                junk = sb("junkb", [128, D], BF16, pb_); junk_b = Buf("junkb")
                xmT2 = [sb(f"xmT{i}", [128, 16, 128], F32, pb_) for i in range(2)]
                xmT2_b = [[Buf(f"xmT{i}_{k}") for k in range(16)] for i in range(2)]
                NSM = 24
                sm = sb("sm", [128, NSM, 40], F32, pb_)
                sm_b = [Buf(f"sm{i}") for i in range(NSM)]
                m8 = sb("m8", [128, 8], F32, pb_); m8_b = Buf("m8")
                i8 = sb("i8", [128, 8], U32, pb_); i8_b = Buf("i8")
                Mb = sb("Mb", [128, 32], BF16, pb_); Mb_b = Buf("Mb")
                sidx = sb("sidx", [128, 2], I32, pb_); sidx_b = Buf("sidx")
                pk = [sb(f"pk{i}", [128, 2], F32, pb_) for i in range(2)]
                pk_b = [Buf(f"pk{i}") for i in range(2)]
                stab_w = Buf("slot_tab_w")
                Sx.dma("sp", lambda: SP.dma_start(out=gfb[:], in_=gffn), [], [gfb_b], gfb_b)
                mixv = mixT_scr.rearrange("c p t -> p c t")
                def XB(t, ms):
                    tt = t % 4
                    gt = s * 16 + t
                    sl = t % 2
                    r0 = s * S + t * 128
                    Sx.dma("sp", lambda: SP.dma_start(out=xin[sl][:], in_=x[r0:r0 + 128, :]), [], [xin_b[sl]], xin_b[sl])
                    for cg in range(4):
                        pt, pbk = bank()
                        cs_ = slice(cg * 512, (cg + 1) * 512)
                        mm_group(pt[:], pbk, [(mixg[ms][:, kc, tt * 128:(tt + 1) * 128], wo[:, kc, cs_]) for kc in range(16)],
                                 [mixg_b[ms]] + wo_b)
                        Sx.op("dve", lambda: V.tensor_tensor(out=xm[sl][:, cs_], in0=pt[:], in1=xin[sl][:, cs_], op=ALU.add),
                              [pbk, xin_b[sl]], [xm_b[sl]])
                    xb = xmid_b[t % 2]
                    Sx.dma("sp", lambda: SP.dma_start(out=xmid_scr[r0:r0 + 128, :], in_=xm[sl][:]), [xm_b[sl]], [xb], xb)
                    ss = sm[:, 19 + t % 4, 0:1]
                    Sx.op("act", lambda: A.activation(out=junk[:], in_=xm[sl][:], func=AF.Square, accum_out=ss), [xm_b[sl]], [junk_b, sm_b[19 + t % 4]])
                    rstd_from_ss(ss, sm_b[19 + t % 4], D)
                    Sx.op("dve", lambda: V.scalar_tensor_tensor(out=h2b[sl][:], in0=xm[sl][:], scalar=ss, in1=gfb[:], op0=ALU.mult, op1=ALU.mult),
                          [xm_b[sl], sm_b[19 + t % 4], gfb_b], [h2b_b[sl]])
                    hb_ = h2s_b[t % 2]
                    Sx.dma("sp", lambda: SP.dma_start(out=h2_scr[r0:r0 + 128, :], in_=h2b[sl][:]), [h2b_b[sl]], [hb_], hb_)

                def RB(t):
                    gt = s * 16 + t
                    sl = t % 2
                    xmT, xmT_b = xmT2[t % 2], xmT2_b[t % 2]
                    ss = sm[:, 19 + t % 4, 0:1]
                    for q in range(4):
                        pt, pbk = bank()
                        def fn():
                            last = None
                            for j in range(4):
                                kc = q * 4 + j
                                last = T.transpose(out=pt[:, j * 128:(j + 1) * 128], in_=xm[sl][:, kc * 128:(kc + 1) * 128], identity=identf[:])
                            return last
                        Sx.op("pe", fn, [xm_b[sl], identf_b], [pbk])
                        for j in range(4):
                            kc = q * 4 + j
                            if q % 2 == 0:
                                Sx.op("dve", lambda: V.tensor_scalar(out=xmT[:, kc, :], in0=pt[:, j * 128:(j + 1) * 128],
                                                                     scalar1=pv[:, PV_GFFN + kc:PV_GFFN + kc + 1], scalar2=None, op0=ALU.mult),
                                      [pbk, pv_b], [xmT_b[kc]])
                            else:
                                Sx.op("act", lambda: A.activation(out=xmT[:, kc, :], in_=pt[:, j * 128:(j + 1) * 128], func=AF.Copy,
                                                                  scale=pv[:, PV_GFFN + kc:PV_GFFN + kc + 1]), [pbk, pv_b], [xmT_b[kc]])
                    yield
                    pl, pl_b = bank()
                    mm_group(pl[:, 0:36], pl_b, [(xmT[:, kc, :], wrs[:, kc, :]) for kc in range(16)], xmT_b + [wrs_b])
                    lg = sm[:, 1, 0:36]
                    Sx.op("dve", lambda: V.scalar_tensor_tensor(out=lg, in0=pl[:, 0:36], scalar=ss, in1=brs[:], op0=ALU.mult, op1=ALU.add),
                          [pl_b, sm_b[19 + t % 4], brs_b], [sm_b[1]])
                    gmax = sm[:, 2, 0:1]
                    Sx.op("dve", lambda: V.tensor_reduce(out=gmax, in_=sm[:, 1, 0:4], axis=AX.X, op=ALU.max), [sm_b[1]], [sm_b[2]])
                    ngmax = sm[:, 3, 0:1]
                    Sx.op("dve", lambda: V.tensor_scalar(out=ngmax, in0=gmax, scalar1=-1.0, scalar2=None, op0=ALU.mult), [sm_b[2]], [sm_b[3]])
                    gexp = sm[:, 4, 0:4]
                    Sx.op("act", lambda: A.activation(out=gexp, in_=sm[:, 1, 0:4], func=AF.Exp, bias=ngmax), [sm_b[1], sm_b[3]], [sm_b[4]])
                    gp = sm[:, 5, 0:1]
                    Sx.op("dve", lambda: V.tensor_reduce(out=gp, in_=gexp, axis=AX.X, op=ALU.add), [sm_b[4]], [sm_b[5]])
                    Sx.op("dve", lambda: V.reciprocal(out=gp, in_=gp), [sm_b[5]], [sm_b[5]])
                    pen = sm[:, 6, 0:4]
                    Sx.op("dve", lambda: V.tensor_scalar(out=pen, in0=sm[:, 1, 0:4], scalar1=gmax, scalar2=None, op0=ALU.is_ge), [sm_b[1], sm_b[2]], [sm_b[6]])
                    Sx.op("dve", lambda: V.tensor_scalar(out=pen, in0=pen, scalar1=1e9, scalar2=-1e9, op0=ALU.mult, op1=ALU.add), [sm_b[6]], [sm_b[6]])
                    el = sm[:, 7, 0:32]
                    for g4 in range(4):
                        Sx.op("dve", lambda: V.tensor_scalar(out=sm[:, 7, g4 * 8:(g4 + 1) * 8], in0=sm[:, 1, 4 + g4 * 8:4 + (g4 + 1) * 8],
                                                             scalar1=sm[:, 6, g4:g4 + 1], scalar2=None, op0=ALU.add), [sm_b[1], sm_b[6]], [sm_b[7]])
                    Sx.op("dve", lambda: V.max(out=m8[:], in_=el), [sm_b[7]], [m8_b])
                    Sx.op("dve", lambda: V.max_index(out=i8[:], in_max=m8[:], in_values=el), [sm_b[7], m8_b], [i8_b])
                    ef = sm[:, 8, 0:2]
                    Sx.op("dve", lambda: V.tensor_copy(out=ef, in_=i8[:, 0:2]), [i8_b], [sm_b[8]])
                    ed = sm[:, 9, 0:1]
                    Sx.op("dve", lambda: V.tensor_tensor(out=ed, in0=m8[:, 1:2], in1=m8[:, 0:1], op=ALU.subtract), [m8_b], [sm_b[9]])
                    Sx.op("act", lambda: A.activation(out=ed, in_=ed, func=AF.Exp), [sm_b[9]], [sm_b[9]])
                    w12 = sm[:, 10, 0:2]
                    Sx.op("dve", lambda: V.tensor_scalar(out=sm[:, 10, 0:1], in0=ed, scalar1=1.0, scalar2=None, op0=ALU.add), [sm_b[9]], [sm_b[10]])
                    Sx.op("dve", lambda: V.reciprocal(out=sm[:, 10, 0:1], in_=sm[:, 10, 0:1]), [sm_b[10]], [sm_b[10]])
                    Sx.op("dve", lambda: V.tensor_tensor(out=sm[:, 10, 1:2], in0=sm[:, 10, 0:1], in1=ed, op=ALU.mult), [sm_b[10], sm_b[9]], [sm_b[10]])
                    Sx.op("dve", lambda: V.tensor_scalar(out=w12, in0=w12, scalar1=gp, scalar2=None, op0=ALU.mult), [sm_b[10], sm_b[5]], [sm_b[10]])
                    M1, M2 = sm[:, 11, 0:32], sm[:, 12, 0:32]
                    Sx.op("dve", lambda: V.tensor_scalar(out=M1, in0=iota_e[:], scalar1=sm[:, 8, 0:1], scalar2=None, op0=ALU.is_equal), [iota_e_b, sm_b[8]], [sm_b[11]])
                    Sx.op("dve", lambda: V.tensor_scalar(out=M2, in0=iota_e[:], scalar1=sm[:, 8, 1:2], scalar2=None, op0=ALU.is_equal), [iota_e_b, sm_b[8]], [sm_b[12]])
                    Sx.op("dve", lambda: V.tensor_tensor(out=Mb[:], in0=M1, in1=M2, op=ALU.add), [sm_b[11], sm_b[12]], [Mb_b])
                    yield
                    pp, pp_b = bank()
                    mm_group(pp[:, 0:32], pp_b, [(ustr[:], Mb[:]), (onefull[:], Rb[:])], [ustr_b, Mb_b, onefull_b, Rb_b])
                    pos = sm[:, 13, 0:32]
                    Sx.op("act", lambda: A.activation(out=pos, in_=pp[:, 0:32], func=AF.Copy), [pp_b], [sm_b[13]])
                    Sx.op("dve", lambda: V.tensor_tensor(out=Rb[:], in0=Rb[:], in1=Mb[:], op=ALU.add), [Rb_b, Mb_b], [Rb_b])
                    p12 = sm[:, 14, 0:2]
                    for k, Mk in enumerate((M1, M2)):
                        Sx.op("dve", lambda: V.tensor_tensor(out=sm[:, 15 + k, 0:32], in0=Mk, in1=pos, op=ALU.mult), [sm_b[11 + k], sm_b[13]], [sm_b[15 + k]])
                        Sx.op("dve", lambda: V.tensor_reduce(out=sm[:, 14, k:k + 1], in_=sm[:, 15 + k, 0:32], axis=AX.X, op=ALU.add), [sm_b[15 + k]], [sm_b[14]])
                    slot = sm[:, 17, 0:2]
                    Sx.op("dve", lambda: V.scalar_tensor_tensor(out=slot, in0=ef, scalar=float(CAP), in1=p12, op0=ALU.mult, op1=ALU.add), [sm_b[8], sm_b[14]], [sm_b[17]])
                    vld = sm[:, 18, 0:2]
                    Sx.op("dve", lambda: V.tensor_single_scalar(out=vld, in_=p12, scalar=float(CAP), op=ALU.is_lt), [sm_b[14]], [sm_b[18]])
                    Sx.op("dve", lambda: V.tensor_scalar(out=slot, in0=slot, scalar1=dummyp[:, 0:1], scalar2=None, op0=ALU.subtract), [sm_b[17], dummyp_b], [sm_b[17]])
                    Sx.op("dve", lambda: V.tensor_tensor(out=slot, in0=slot, in1=vld, op=ALU.mult), [sm_b[17], sm_b[18]], [sm_b[17]])
                    Sx.op("dve", lambda: V.tensor_scalar(out=slot, in0=slot, scalar1=dummyp[:, 0:1], scalar2=None, op0=ALU.add), [sm_b[17], dummyp_b], [sm_b[17]])
                    Sx.op("dve", lambda: V.tensor_copy(out=sidx[:], in_=slot), [sm_b[17]], [sidx_b])
                    Sx.op("dve", lambda: V.tensor_copy(out=gix[:, gt, :], in_=slot), [sm_b[17]], [gix_b])
                    for k in range(2):
                        Sx.op("dve", lambda: V.tensor_copy(out=pk[k][:, 0:1], in_=tokid[:, gt:gt + 1]), [tokid_b], [pk_b[k]])
                        Sx.op("dve", lambda: V.tensor_copy(out=pk[k][:, 1:2], in_=sm[:, 10, k:k + 1]), [sm_b[10], pk_b[k]], [pk_b[k]])
                        Sx.dma("pool", lambda: G.indirect_dma_start(out=slot_tab, out_offset=bass.IndirectOffsetOnAxis(ap=sidx[:, k:k + 1], axis=0),
                                                                    in_=pk[k][:], in_offset=None),
                               [pk_b[k], sidx_b], [stab_w], stab_w)

                pend_r = []
                for tg in range(4):
                    ms = tg % 2
                    Sx.dma("sp", lambda: SP.dma_start(out=mixg[ms][:], in_=mixv[:, :, tg * 512:(tg + 1) * 512]), mixs_b, [mixg_b[ms]], mixg_b[ms])
                    for tt in range(4):
                        t = tg * 4 + tt
                        XB(t, ms)
                        order_ = ([pend_r[-1]] + pend_r[:-1]) if pend_r else []
                        for g_ in order_:
                            if next(g_, "done") == "done":
                                pend_r.remove(g_)
                        pend_r.append(RB(t))
                for g_ in pend_r:
                    while next(g_, "done") != "done":
                        pass
                Sx.barrier()

        if debug == "xmid":
            db_ = Buf("dbgx")
            Sx.dma("sp", lambda: [SP.dma_start(out=dbg["xmid"], in_=xmid_scr), SP.dma_start(out=dbg["h2"], in_=h2_scr),
                                  SP.dma_start(out=dbg["stab"], in_=slot_tab),
                                  SP.dma_start(out=dbg["gix"], in_=gix[:].rearrange("p a b -> p (a b)"))], [gix_b], [db_], db_)
            Sx.barrier()
            stop[0] = True
        NJ = CAP // 128
        with ExitStack() as pc:
          if not stop[0]:
            NR = 6
            psb = [pc.enter_context(nc.psum_tensor(f"psb{i}", [128, 1024], BF16)) for i in range(2)]
            psb_b = [Buf(f"psb{i}") for i in range(2)]
            ring = [sb(f"ring{i}", [128, 8192], BF16, pc) for i in range(NR)]
            ring_b = [Buf(f"ring{i}") for i in range(NR)]
            rr = [0]
            stt = [sb(f"stt{i}", [128, NJ, 2], F32, pc) for i in range(2)]
            stt_b = [Buf(f"stt{i}") for i in range(2)]
            tix = [sb(f"tix{i}", [128, NJ], I32, pc) for i in range(2)]
            tix_b = [Buf(f"tix{i}") for i in range(2)]
            xg = [[sb(f"xg{i}_{j}", [128, D], BF16, pc) for j in range(NJ)] for i in range(2)]
            xg_b = [[Buf(f"xg{i}_{j}") for j in range(NJ)] for i in range(2)]
            XT = [sb(f"XT{i}", [128, 16, CAP], BF16, pc) for i in range(2)]
            XT_b = [Buf(f"XT{i}") for i in range(2)]
            HT = [sb(f"HT{i}", [128, 8, CAP], BF16, pc) for i in range(2)]
            HT_b = [Buf(f"HT{i}") for i in range(2)]
            sgt = [sb(f"sgt{i}", [128, CAP], F32, pc) for i in range(2)]
            sgt_b = [Buf(f"sgt{i}") for i in range(2)]
            yst = [sb(f"yst{i}", [128, D], F32, pc) for i in range(2)]
            yst_b = [Buf(f"yst{i}") for i in range(2)]
            yr = [0]
            stab_r = Buf("slot_tab_r")

            def prep(e):
                b = e % 2
                Sx.dma("sp", lambda: SP.dma_start(out=stt[b][:], in_=slot_tab[e * CAP:(e + 1) * CAP, :].rearrange("(j p) c -> p j c", p=128)),
                       [], [stt_b[b]], stt_b[b])
                Sx.op("dve", lambda: V.tensor_copy(out=tix[b][:], in_=stt[b][:, :, 0]), [stt_b[b]], [tix_b[b]])
                for j in range(NJ):
                    Sx.dma("pool", lambda: G.indirect_dma_start(out=xg[b][j][:], out_offset=None, in_=h2_scr,
                                                                in_offset=bass.IndirectOffsetOnAxis(ap=tix[b][:, j:j + 1], axis=0)),
                           [tix_b[b]], [xg_b[b][j]], xg_b[b][j])

            def rv(i, k):
                return ring[i][:].rearrange("p (k n) -> p k n", k=k)

            def load_gu(e, half):
                for base, src in ((0, w_gate), (2, w_up)):
                    i = base + half
                    v = src[e].rearrange("(kc p) n -> p kc n", p=128)
                    Sx.dma("pool", lambda: G.dma_start(out=rv(i, 16), in_=v[:, :, half * 512:(half + 1) * 512]),
                           [], [ring_b[i]], ring_b[i])

            def load_d(e, half):
                i = 4 + half
                v = w_down[e].rearrange("(kc p) n -> p kc n", p=128)
                Sx.dma("pool", lambda: [G.dma_start(out=ring[i][:, k4 * 2048:(k4 + 1) * 2048].rearrange("p (a n) -> p a n", a=2),
                                                    in_=v[:, half * 4 + k4, :].rearrange("p (a n) -> p a n", a=2)) for k4 in range(4)],
                       [], [ring_b[i]], ring_b[i])

            prep(0)
            load_gu(0, 0); load_gu(0, 1); load_d(0, 0); load_d(0, 1)
            g_w = [(rv(0, 16), ring_b[0]), (rv(1, 16), ring_b[1])]
            u_w = [(rv(2, 16), ring_b[2]), (rv(3, 16), ring_b[3])]
            d_w = [(rv(4, 4), ring_b[4]), (rv(5, 4), ring_b[5])]
            for e in range(NE):
                b = e % 2
                if e + 1 < NE:
                    prep(e + 1)
                for j in range(NJ):
                    for hf in range(2):
                        pt, pbk = psb[hf], psb_b[hf]
                        def fn():
                            last = None
                            for q in range(8):
                                kc = hf * 8 + q
                                last = T.transpose(out=pt[:, q * 128:(q + 1) * 128], in_=xg[b][j][:, kc * 128:(kc + 1) * 128], identity=identb[:])
                            return last
                        Sx.op("pe", fn, [xg_b[b][j], identb_b], [pbk])
                        eng = "act" if hf == 0 else "dve"
                        if eng == "act":
                            Sx.op("act", lambda: A.activation(out=XT[b][:, hf * 8:(hf + 1) * 8, j * 128:(j + 1) * 128],
                                                              in_=pt[:].rearrange("p (k t) -> p k t", k=8), func=AF.Copy), [pbk], [XT_b[b]])
                        else:
                            Sx.op("dve", lambda: V.tensor_copy(out=XT[b][:, hf * 8:(hf + 1) * 8, j * 128:(j + 1) * 128],
                                                               in_=pt[:].rearrange("p (k t) -> p k t", k=8)), [pbk], [XT_b[b]])
                for mc in range(8):
                    half, ml = mc // 4, mc % 4
                    pg, pg_b = bank()
                    mm_group(pg[:, 0:CAP], pg_b, [(g_w[half][0][:, kc, ml * 128:(ml + 1) * 128], XT[b][:, kc, :]) for kc in range(16)],
                             [g_w[half][1], XT_b[b]])
                    pu, pu_b = bank()
                    mm_group(pu[:, 0:CAP], pu_b, [(u_w[half][0][:, kc, ml * 128:(ml + 1) * 128], XT[b][:, kc, :]) for kc in range(16)],
                             [u_w[half][1], XT_b[b]])
                    si = mc % 2
                    Sx.op("act", lambda: A.activation(out=sgt[si][:], in_=pg[:, 0:CAP], func=AF.Silu), [pg_b], [sgt_b[si]])
                    Sx.op("dve", lambda: V.tensor_tensor(out=HT[b][:, mc, :], in0=pu[:, 0:CAP], in1=sgt[si][:], op=ALU.mult), [pu_b, sgt_b[si]], [HT_b[b]])
                    if ml == 3 and e + 1 < NE:
                        load_gu(e + 1, half)
                for j in range(NJ):
                    yi = yr[0] % 2
                    yr[0] += 1
                    for cg in range(4):
                        pt, pbk = bank()
                        cs_ = slice(cg * 512, (cg + 1) * 512)
                        mm_group(pt[:], pbk, [(HT[b][:, mc, j * 128:(j + 1) * 128], d_w[mc // 4][0][:, mc % 4, cs_]) for mc in range(8)],
                                 [HT_b[b], d_w[0][1], d_w[1][1]])
                        if cg % 2 == 0:
                            Sx.op("act", lambda: A.activation(out=yst[yi][:, cs_], in_=pt[:], func=AF.Copy, scale=stt[b][:, j, 1:2]), [pbk, stt_b[b]], [yst_b[yi]])
                        else:
                            Sx.op("dve", lambda: V.tensor_scalar(out=yst[yi][:, cs_], in0=pt[:], scalar1=stt[b][:, j, 1:2], scalar2=None, op0=ALU.mult),
                                  [pbk, stt_b[b]], [yst_b[yi]])
                    r0 = e * CAP + j * 128
                    yb = ysl_b[yi]
                    Sx.dma("sp", lambda: SP.dma_start(out=yslot_scr[r0:r0 + 128, :], in_=yst[yi][:]), [yst_b[yi]], [yb], yb)
                if e + 1 < NE:
                    load_d(e + 1, 0); load_d(e + 1, 1)
            Sx.barrier()

        if debug == "moe":
            db_ = Buf("dbgy")
            Sx.dma("sp", lambda: SP.dma_start(out=dbg["ys"], in_=yslot_scr), [], [db_], db_)
            Sx.barrier()
            stop[0] = True
        with ExitStack() as pd:
          if not stop[0]:
            xf = [sb(f"xf{i}", [128, D], F32, pd) for i in range(3)]
            xf_b = [Buf(f"xf{i}") for i in range(3)]
            y1 = [sb(f"y1{i}", [128, D], F32, pd) for i in range(3)]
            y1_b = [Buf(f"y1{i}") for i in range(3)]
            y2 = [sb(f"y2{i}", [128, D], F32, pd) for i in range(3)]
            y2_b = [Buf(f"y2{i}") for i in range(3)]
            ot = [sb(f"ot{i}", [128, D], F32, pd) for i in range(3)]
            ot_b = [Buf(f"ot{i}") for i in range(3)]
            gfn = sb("gfn", [128, D], F32, pd); gfn_b = Buf("gfn")
            junk = sb("junkd", [128, D], BF16, pd); junk_b = Buf("junkd")
            ssd = [sb(f"ssd{i}", [128, 1], F32, pd) for i in range(3)]
            ssd_b = [Buf(f"ssd{i}") for i in range(3)]
            out_b = [Buf(f"out{i}") for i in range(3)]
            Sx.dma("sp", lambda: SP.dma_start(out=gfn[:], in_=gfin), [], [gfn_b], gfn_b)
            for gt in range(32):
                sl = gt % 3
                r0 = gt * 128
                Sx.dma("sp", lambda: SP.dma_start(out=xf[sl][:], in_=xmid_scr[r0:r0 + 128, :]), [], [xf_b[sl]], xf_b[sl])
                Sx.dma("pool", lambda: G.indirect_dma_start(out=y1[sl][:], out_offset=None, in_=yslot_scr,
                                                            in_offset=bass.IndirectOffsetOnAxis(ap=gix[:, gt, 0:1], axis=0)),
                       [gix_b], [y1_b[sl]], y1_b[sl])
                Sx.dma("pool", lambda: G.indirect_dma_start(out=y2[sl][:], out_offset=None, in_=yslot_scr,
                                                            in_offset=bass.IndirectOffsetOnAxis(ap=gix[:, gt, 1:2], axis=0)),
                       [gix_b], [y2_b[sl]], y2_b[sl])
                Sx.op("dve", lambda: V.tensor_tensor(out=xf[sl][:], in0=xf[sl][:], in1=y1[sl][:], op=ALU.add), [xf_b[sl], y1_b[sl]], [xf_b[sl]])
                Sx.op("dve", lambda: V.tensor_tensor(out=xf[sl][:], in0=xf[sl][:], in1=y2[sl][:], op=ALU.add), [xf_b[sl], y2_b[sl]], [xf_b[sl]])
                Sx.op("act", lambda: A.activation(out=junk[:], in_=xf[sl][:], func=AF.Square, accum_out=ssd[sl][:]), [xf_b[sl]], [junk_b, ssd_b[sl]])
                rstd_from_ss(ssd[sl][:], ssd_b[sl], D)
                Sx.op("dve", lambda: V.scalar_tensor_tensor(out=ot[sl][:], in0=xf[sl][:], scalar=ssd[sl][:, 0:1], in1=gfn[:], op0=ALU.mult, op1=ALU.mult),
                      [xf_b[sl], ssd_b[sl], gfn_b], [ot_b[sl]])
                Sx.dma("sp", lambda: SP.dma_start(out=out[r0:r0 + 128, :], in_=ot[sl][:]), [ot_b[sl]], [out_b[sl]], out_b[sl])
            Sx.barrier()
    return nc


def _const_tables():
    half = 64
    inv = (10000.0 ** (-np.arange(half, dtype=np.float32) / half)).astype(np.float32)
    ang = np.arange(S, dtype=np.float32)[None, :] * inv[:, None]
    cos = np.cos(ang).astype(np.float32)
    sin = np.sin(ang).astype(np.float32)
    cst = np.stack([np.concatenate([cos, cos], 0), np.concatenate([-sin, sin], 0)], 0)
    hh = np.arange(8, dtype=np.float64)
    log_g = np.log1p(-(2.0 ** (-5.0 - hh)))
    sc = 128.0 ** -0.5
    jj = np.arange(128, dtype=np.float64)
    ii = np.arange(512, dtype=np.float64)
    cf = np.exp(log_g[:, None] * ii[None, :])
    cfq = np.broadcast_to(cf[:, None, :], (8, 128, 512))
    dl = np.arange(16, dtype=np.float64)
    ct = np.exp(log_g[:, None, None] * (128.0 * dl[None, None, :] - jj[None, :, None])) * sc
    ctab = np.zeros((128, 128), np.float64)
    for h_ in range(8):
        ctab[:, h_ * 16:(h_ + 1) * 16] = ct[h_]
    rr_ = np.arange(4, dtype=np.float64)
    thr = 128.0 * rr_[None, :, None] + jj[:, None, None]
    valid = ii[None, None, :] >= thr
    dm = np.where(valid[None], np.exp(-log_g[:, None, None, None] * thr[None]) * sc, 0.0)
    strips = (cfq, ctab, dm.reshape(8, 128, 2048))
    return np.ascontiguousarray(cst, dtype=np.float32), tuple(np.ascontiguousarray(a_, dtype=np.float32) for a_ in strips)


def _w_in_cols():
    R = 1024
    cols = []
    for p in range(4):
        h0, h1 = 2 * p, 2 * p + 1
        for base in (0, R):
            for h in (h0, h1):
                cols += list(range(base + h * 128, base + h * 128 + 128))
        cols += list(range(2 * R + h0 * 128, 2 * R + h0 * 128 + 256))
        cols += list(range(3 * R + h0 * 128, 3 * R + h0 * 128 + 128))
        cols += list(range(3 * R + h1 * 128, 3 * R + h1 * 128 + 128))
    for n in range(8):
        cols += list(range(4 * R + n * 128, 4 * R + n * 128 + 128))
        cols += list(range(5 * R + n * 128, 5 * R + n * 128 + 128))
    return np.array(cols)


_NC_CACHE = {}


def kernel(x, norm_mix_g, w_in, ret_norm_g, conv_w, conv_b, w_rg, b_rg, w_ig, b_ig,
           lru_lambda, lru_norm_g, w_out, norm_ffn_g, w_group, b_group, w_router, b_router,
           w_gate, w_up, w_down, norm_final_g):
    f = lambda a: np.ascontiguousarray(np.asarray(a), dtype=np.float32)
    x = f(x).reshape(NCORES, TPC, D)
    wi = f(w_in)[0][:, _w_in_cols()]
    wi = np.ascontiguousarray(wi.reshape(16, 128, 48, 128).transpose(2, 1, 0, 3)).reshape(48, 128, 2048)
    pvec = np.zeros((128, 128), np.float32)
    pvec[:, 0:16] = f(norm_mix_g)[0].reshape(16, 128).T
    pvec[:, 16:32] = f(norm_ffn_g)[0].reshape(16, 128).T
    pvec[:, 32:40] = f(ret_norm_g)[0].reshape(8, 128).T
    cw = f(conv_w)[0]
    for n in range(8):
        for k in range(4):
            pvec[:, 40 + n * 4 + k] = cw[k, n * 128:(n + 1) * 128]
    pvec[:, 72:80] = f(conv_b)[0].reshape(8, 128).T
    pvec[:, 80:88] = f(b_rg)[0].reshape(8, 128).T
    pvec[:, 88:96] = f(b_ig)[0].reshape(8, 128).T
    pvec[:, 96:104] = f(lru_lambda)[0].reshape(8, 128).T
    pvec[:, 104:112] = f(lru_norm_g)[0].reshape(8, 128).T
    wgates = np.ascontiguousarray(np.concatenate([f(w_rg)[0], f(w_ig)[0]], 0))
    wr = np.ascontiguousarray(np.concatenate([f(w_group)[0], f(w_router)[0]], 1))
    brow = np.ascontiguousarray(np.broadcast_to(np.concatenate([f(b_group)[0], f(b_router)[0]])[None, :], (128, 36)))
    gfin = np.ascontiguousarray(np.broadcast_to(f(norm_final_g)[None, :], (128, D)))
    gffn = np.ascontiguousarray(np.broadcast_to(f(norm_ffn_g)[0][None, :], (128, D)))
    cst, strips = _const_tables()
    shared = {"w_in": wi, "w_out": f(w_out)[0], "pvec": pvec, "wgates": wgates, "wr": wr, "brow": brow,
              "gfin": gfin, "gffn": gffn, "cst": cst, "cfq": strips[0], "ctab": strips[1], "dmask": strips[2],
              "w_gate": f(w_gate)[0], "w_up": f(w_up)[0], "w_down": f(w_down)[0]}
    if "nc" not in _NC_CACHE:
        _NC_CACHE["nc"] = build_program()
    nc = _NC_CACHE["nc"]
    in_maps = [dict(shared, x=x[c]) for c in range(NCORES)]
    res = run_bass_kernel_spmd(nc, in_maps, core_ids=list(range(NCORES)))
    out = np.stack([np.asarray(r["out"], dtype=np.float32) for r in res.results], 0)
    return out.reshape(16, S, D)
```

```python
from contextlib import ExitStack
import math
import numpy as np
import concourse.bass as bass
import concourse.mybir as mybir
from concourse.bass_utils import run_bass_kernel_spmd

F32 = mybir.dt.float32
BF16 = mybir.dt.bfloat16
I32 = mybir.dt.int32
U32 = mybir.dt.uint32
AF = mybir.ActivationFunctionType
ALU = mybir.AluOpType
AX = mybir.AxisListType

NCORES = 8
D = 2048
S = 2048
TPC = 4096
NE = 32
CAP = 384
NS = NE * CAP
EPS = 1e-6
STRIPW = 384 + 2048
GK = 1.5957691216057308


class Buf:
    __slots__ = ("name", "last_w", "readers", "sem", "semcnt")

    def __init__(self, name):
        self.name = name
        self.last_w = None
        self.readers = {}
        self.sem = None
        self.semcnt = 0


class Sched:
    def __init__(self, nc, stack):
        self.nc = nc
        self.stack = stack
        self.engs = {"pe": nc.tensor, "act": nc.scalar, "dve": nc.vector, "pool": nc.gpsimd, "sp": nc.sync}
        self.esem = {k: stack.enter_context(nc.semaphore("e_" + k)) for k in self.engs}
        self.ecnt = {k: 0 for k in self.engs}
        self.known = {k: {} for k in self.engs}
        self.dsems = []
        self.nsem = len(self.engs)

    def _wait(self, eng, ev, skip_sem=None):
        if ev is None:
            return
        sem, val = ev
        if skip_sem is not None and sem is skip_sem:
            return
        kn = self.known[eng]
        if kn.get(id(sem), 0) >= val:
            return
        self.engs[eng].wait_ge(sem, val)
        kn[id(sem)] = val

    def _deps(self, eng, reads, writes, fifo):
        skip = self.esem[eng] if fifo else None
        for b in reads:
            self._wait(eng, b.last_w, skip)
        for b in writes:
            self._wait(eng, b.last_w, skip)
            for sem_id, ev in list(b.readers.items()):
                self._wait(eng, ev, skip)

    def _book(self, ev, reads, writes):
        for b in reads:
            b.readers[id(ev[0])] = ev
        for b in writes:
            b.last_w = ev
            b.readers = {}

    def op(self, eng, fn, reads=(), writes=(), fifo=False):
        self._deps(eng, reads, writes, fifo)
        inst = fn()
        self.ecnt[eng] += 1
        ev = (self.esem[eng], self.ecnt[eng])
        inst.then_inc(ev[0], 1)
        self._book(ev, reads, writes)
        return ev

    def dma(self, eng, fn, reads, writes, dst):
        self._deps(eng, reads, writes, False)
        if dst.sem is None:
            dst.sem = self.stack.enter_context(self.nc.semaphore(f"d{self.nsem}_" + dst.name))
            self.dsems.append(dst)
            self.nsem += 1
        insts = fn()
        if not isinstance(insts, (list, tuple)):
            insts = [insts]
        for inst in insts:
            inst.then_inc(dst.sem, 16)
            dst.semcnt += 16
        ev = (dst.sem, dst.semcnt)
        self._book(ev, reads, writes)
        return ev

    def barrier(self, engines=None):
        for e in (engines or self.engs):
            for o in self.engs:
                if o != e and self.ecnt[o] > 0:
                    self._wait(e, (self.esem[o], self.ecnt[o]))
            for b in self.dsems:
                self._wait(e, (b.sem, b.semcnt))


def build_program(debug=None):
    nc = bass.Bass("TRN2", target_bir_lowering=False)
    T, V, A, G, SP = nc.tensor, nc.vector, nc.scalar, nc.gpsimd, nc.sync

    def din(name, shape, dt=F32):
        return nc.dram_tensor(name, shape, dt, kind="ExternalInput").ap()

    x = din("x", [TPC, D])
    w_in = din("w_in", [48, 128, 16 * 128])
    w_out = din("w_out", [D, D])
    pvec = din("pvec", [128, 128])
    wgates = din("wgates", [16, 128, 128])
    wr = din("wr", [D, 36])
    brow = din("brow", [128, 36])
    gfin = din("gfin", [128, D])
    gffn = din("gffn", [128, D])
    cst = din("cst", [2, 128, S])
    dmask = din("dmask", [8, 128, 4 * 512])
    cfq = din("cfq", [8, 128, 512])
    ctab = din("ctab", [128, 128])
    w_gate = din("w_gate", [NE, D, 1024])
    w_up = din("w_up", [NE, D, 1024])
    w_down = din("w_down", [NE, 1024, D])
    out = nc.dram_tensor("out", [TPC, D], F32, kind="ExternalOutput").ap()

    def dscr(name, shape, dt):
        return nc.dram_tensor(name, shape, dt, kind="Internal").ap()

    mixT_scr = dscr("mixT_scr", [16, 128, S], BF16)
    xmid_scr = dscr("xmid_scr", [TPC, D], F32)
    h2_scr = dscr("h2_scr", [TPC + 128, D], BF16)
    slot_tab = dscr("slot_tab", [NS + 128, 2], F32)
    yslot_scr = dscr("yslot_scr", [NS + 128, D], F32)

    dbg = {}
    if debug == "mix":
        dbg["mix"] = nc.dram_tensor("dbg_mix", [16, 128, S], BF16, kind="ExternalOutput").ap()
    if debug == "xmid":
        dbg["xmid"] = nc.dram_tensor("dbg_xmid", [TPC, D], F32, kind="ExternalOutput").ap()
        dbg["h2"] = nc.dram_tensor("dbg_h2", [TPC + 128, D], BF16, kind="ExternalOutput").ap()
        dbg["stab"] = nc.dram_tensor("dbg_stab", [NS + 128, 2], F32, kind="ExternalOutput").ap()
        dbg["gix"] = nc.dram_tensor("dbg_gix", [128, 64], I32, kind="ExternalOutput").ap()
        dbg["lg"] = nc.dram_tensor("dbg_lg", [128, 24 * 40], F32, kind="ExternalOutput").ap()
    if debug == "moe":
        dbg["ys"] = nc.dram_tensor("dbg_ys", [NS + 128, D], F32, kind="ExternalOutput").ap()
    stop = [False]
    PV_GMIX, PV_GFFN, PV_GRET, PV_CW, PV_CB, PV_BRG, PV_BIG, PV_LAM, PV_GLRU = 0, 16, 32, 40, 72, 80, 88, 96, 104

    with ExitStack() as top:
        Sx = Sched(nc, top)
        uniq = [0]

        def sb(name, shape, dt, st=top):
            uniq[0] += 1
            return st.enter_context(nc.sbuf_tensor(f"{name}_{uniq[0]}", shape, dt))

        NPS = 6
        psf = [top.enter_context(nc.psum_tensor(f"psf{i}", [128, 512], F32)) for i in range(NPS)]
        psf_b = [Buf(f"psf{i}") for i in range(NPS)]
        gen_ring = [0, 1, 2, 3]
        ps_rr = [0]

        acc_rr = [0]

        def bank():
            i = gen_ring[ps_rr[0] % len(gen_ring)]
            ps_rr[0] += 1
            return psf[i], psf_b[i]

        def accbank():
            i = 4 + acc_rr[0] % 2
            acc_rr[0] += 1
            return psf[i], psf_b[i]

        pv = sb("pv", [128, 128], F32); pv_b = Buf("pv")
        identf = sb("identf", [128, 128], F32); identf_b = Buf("identf")
        identb = sb("identb", [128, 128], BF16); identb_b = Buf("identb")
        onesb = sb("onesb", [128, 128], BF16); onesb_b = Buf("onesb")
        onefull = sb("onefull", [128, 128], BF16); onefull_b = Buf("onefull")
        ustr = sb("ustr", [128, 128], BF16); ustr_b = Buf("ustr")
        iota_e = sb("iota_e", [128, 32], F32); iota_e_b = Buf("iota_e")
        dummyp = sb("dummyp", [128, 1], F32); dummyp_b = Buf("dummyp")
        tokid = sb("tokid", [128, 32], F32); tokid_b = Buf("tokid")
        gix = sb("gix", [128, 32, 2], I32); gix_b = Buf("gix")
        Rb = sb("Rb", [128, 32], BF16); Rb_b = Buf("Rb")
        wrs = sb("wrs", [128, 16, 36], F32); wrs_b = Buf("wrs")
        brs = sb("brs", [128, 36], F32); brs_b = Buf("brs")
        wg_sb = sb("wg_sb", [128, 16, 128], BF16); wg_b = Buf("wg_sb")
        scl = sb("scl", [128, 8], F32); scl_b = Buf("scl")
        tmpi = sb("tmpi", [128, 128], I32); tmpi_b = Buf("tmpi")
        tmpf = sb("tmpf", [128, 128], F32); tmpf_b = Buf("tmpf")

        Sx.dma("sp", lambda: SP.dma_start(out=pv[:], in_=pvec), [], [pv_b], pv_b)
        Sx.dma("sp", lambda: SP.dma_start(out=wrs[:], in_=wr.rearrange("(kc p) n -> p kc n", p=128)), [], [wrs_b], wrs_b)
        Sx.dma("sp", lambda: SP.dma_start(out=brs[:], in_=brow), [], [brs_b], brs_b)
        ctab_sb = sb("ctab_sb", [128, 128], F32); ctab_b = Buf("ctab_sb")
        Sx.dma("sp", lambda: SP.dma_start(out=ctab_sb[:], in_=ctab), [], [ctab_b], ctab_b)
        Sx.dma("pool", lambda: G.dma_start(out=wg_sb[:], in_=wgates.rearrange("n c d -> c n d")), [], [wg_b], wg_b)
        Sx.op("pool", lambda: G.iota(tmpi[:], pattern=[[1, 128]], base=0, channel_multiplier=-1), [], [tmpi_b])
        Sx.op("dve", lambda: V.tensor_copy(out=tmpf[:], in_=tmpi[:]), [tmpi_b], [tmpf_b])
        Sx.op("dve", lambda: V.tensor_single_scalar(out=identf[:], in_=tmpf[:], scalar=0.0, op=ALU.is_equal), [tmpf_b], [identf_b])
        Sx.op("dve", lambda: V.tensor_copy(out=identb[:], in_=identf[:]), [identf_b], [identb_b])
        Sx.op("dve", lambda: V.tensor_single_scalar(out=ustr[:], in_=tmpf[:], scalar=0.0, op=ALU.is_gt), [tmpf_b], [ustr_b])
        permf = sb("permf", [128, 128], F32); permf_b = Buf("permf")
        Sx.op("dve", lambda: V.tensor_single_scalar(out=permf[:], in_=tmpf[:], scalar=64.0, op=ALU.is_equal), [tmpf_b], [permf_b])
        Sx.op("dve", lambda: V.tensor_single_scalar(out=tmpf[:, :], in_=tmpf[:], scalar=-64.0, op=ALU.is_equal), [tmpf_b], [tmpf_b])
        Sx.op("dve", lambda: V.tensor_tensor(out=permf[:], in0=permf[:], in1=tmpf[:], op=ALU.add), [permf_b, tmpf_b], [permf_b])
        Sx.op("dve", lambda: V.memset(onesb[:], 1.0 / 128.0), [], [onesb_b])
        Sx.op("dve", lambda: V.memset(onefull[:], 1.0), [], [onefull_b])
        Sx.op("dve", lambda: V.memset(Rb[:], 0.0), [], [Rb_b])
        Sx.op("pool", lambda: G.iota(tmpi[:, 0:32], pattern=[[1, 32]], base=0, channel_multiplier=0), [tmpf_b], [tmpi_b])
        Sx.op("dve", lambda: V.tensor_copy(out=iota_e[:], in_=tmpi[:, 0:32]), [tmpi_b], [iota_e_b])
        Sx.op("pool", lambda: G.iota(tmpi[:, 0:32], pattern=[[128, 32]], base=0, channel_multiplier=1), [iota_e_b], [tmpi_b])
        Sx.op("dve", lambda: V.tensor_copy(out=tokid[:], in_=tmpi[:, 0:32]), [tmpi_b], [tokid_b])
        Sx.op("pool", lambda: G.iota(tmpi[:, 0:1], pattern=[[0, 1]], base=NS, channel_multiplier=1), [tokid_b], [tmpi_b])
        Sx.op("dve", lambda: V.tensor_copy(out=dummyp[:], in_=tmpi[:, 0:1]), [tmpi_b], [dummyp_b])
        Sx.op("act", lambda: A.activation(out=scl[:], in_=pv[:, PV_LAM:PV_LAM + 8], func=AF.Exp, scale=-1.0), [pv_b], [scl_b])
        Sx.op("act", lambda: A.activation(out=scl[:], in_=scl[:], func=AF.Ln, bias=1.0), [scl_b], [scl_b])
        Sx.op("dve", lambda: V.tensor_scalar(out=scl[:], in0=scl[:], scalar1=-8.0, scalar2=None, op0=ALU.mult), [scl_b], [scl_b])

        with ExitStack() as ini:
            zt = sb("zt", [128, D], F32, ini); zt_b = Buf("zt")
            zb = sb("zb", [128, D], BF16, ini); zb_b = Buf("zb")
            sti = sb("sti", [128, NS // 128, 2], F32, ini); sti_b = Buf("sti")
            h2z_b, ysz_b, stab_b = Buf("h2z"), Buf("ysz"), Buf("stab")
            Sx.op("dve", lambda: V.memset(zt[:], 0.0), [], [zt_b])
            Sx.op("dve", lambda: V.memset(zb[:], 0.0), [], [zb_b])
            Sx.op("dve", lambda: V.memset(sti[:, :, 0:1], float(TPC)), [], [sti_b])
            Sx.op("dve", lambda: V.memset(sti[:, :, 1:2], 0.0), [sti_b], [sti_b])
            Sx.dma("sp", lambda: SP.dma_start(out=h2_scr[TPC:TPC + 128, :], in_=zb[:]), [zb_b], [h2z_b], h2z_b)
            Sx.dma("sp", lambda: SP.dma_start(out=yslot_scr[NS:NS + 128, :], in_=zt[:]), [zt_b], [ysz_b], ysz_b)
            Sx.dma("sp", lambda: SP.dma_start(out=slot_tab[0:NS, :].rearrange("(p r) c -> p r c", p=128), in_=sti[:]),
                   [sti_b], [stab_b], stab_b)
            Sx.barrier()

        xmid_b = [Buf(f"xmid_scr{i}") for i in range(2)]
        h2s_b = [Buf(f"h2_scr{i}") for i in range(2)]
        mixs_b = [Buf(f"mixT_scr{i}") for i in range(4)]
        ysl_b = [Buf(f"yslot_scr{i}") for i in range(2)]

        def rstd_from_ss(ss, ss_b, n_over):
            Sx.op("act", lambda: A.activation(out=ss, in_=ss, func=AF.Ln, scale=1.0 / n_over, bias=EPS), [ss_b], [ss_b])
            Sx.op("act", lambda: A.activation(out=ss, in_=ss, func=AF.Exp, scale=-0.5), [ss_b], [ss_b])

        def mm_group(outp, out_b, pairs, reads):
            def fn():
                last = None
                n = len(pairs)
                for i, (l, r) in enumerate(pairs):
                    last = T.matmul(outp, lhsT=l, rhs=r, start=(i == 0), stop=(i == n - 1))
                return last
            return Sx.op("pe", fn, reads, [out_b])

        for s in range(2):
            with ExitStack() as r1:
                R1 = sb("R1", [128, 16 * 2048], BF16, r1)
                hT = R1[:].rearrange("p (k t) -> p k t", k=16)
                hT_b = [[Buf(f"hT{t}_{k}") for k in range(16)] for t in range(16)]
                with ExitStack() as a1:
                    xin = [sb(f"xin{i}", [128, D], F32, a1) for i in range(2)]
                    xin_b = [Buf(f"xin{i}") for i in range(2)]
                    junk = sb("junk", [128, D], BF16, a1); junk_b = Buf("junk")
                    ssa = [sb(f"ssa{i}", [128, 1], F32, a1) for i in range(2)]
                    ssa_b = [Buf(f"ssa{i}") for i in range(2)]
                    def a1_stats(t):
                        sl = t % 2
                        r0 = s * S + t * 128
                        Sx.dma("sp", lambda: SP.dma_start(out=xin[sl][:], in_=x[r0:r0 + 128, :]), [], [xin_b[sl]], xin_b[sl])
                        Sx.op("act", lambda: A.activation(out=junk[:], in_=xin[sl][:], func=AF.Square, accum_out=ssa[sl][:]),
                              [xin_b[sl]], [junk_b, ssa_b[sl]])
                        rstd_from_ss(ssa[sl][:], ssa_b[sl], D)
                        Sx.op("dve", lambda: V.tensor_scalar(out=xin[sl][:], in0=xin[sl][:], scalar1=ssa[sl][:, 0:1], scalar2=None, op0=ALU.mult),
                              [xin_b[sl], ssa_b[sl]], [xin_b[sl]])

                    def a1_tr(t):
                        sl = t % 2
                        for q in range(4):
                            pt, pb = bank()
                            def fn():
                                last = None
                                for j in range(4):
                                    kc = q * 4 + j
                                    last = T.transpose(out=pt[:, j * 128:(j + 1) * 128], in_=xin[sl][:, kc * 128:(kc + 1) * 128], identity=identf[:])
                                return last
                            Sx.op("pe", fn, [xin_b[sl], identf_b], [pb])
                            for j in range(4):
                                kc = q * 4 + j
                                if q % 2 == 0:
                                    Sx.op("dve", lambda: V.tensor_scalar(out=hT[:, kc, t * 128:(t + 1) * 128], in0=pt[:, j * 128:(j + 1) * 128],
                                                                         scalar1=pv[:, PV_GMIX + kc:PV_GMIX + kc + 1], scalar2=None, op0=ALU.mult),
                                          [pb, pv_b], [hT_b[t][kc]])
                                else:
                                    Sx.op("act", lambda: A.activation(out=hT[:, kc, t * 128:(t + 1) * 128], in_=pt[:, j * 128:(j + 1) * 128],
                                                                      func=AF.Copy, scale=pv[:, PV_GMIX + kc:PV_GMIX + kc + 1]),
                                          [pb, pv_b], [hT_b[t][kc]])

                    a1_stats(0)
                    for t in range(16):
                        if t + 1 < 16:
                            a1_stats(t + 1)
                        a1_tr(t)
                    Sx.barrier()

                with ExitStack() as a2:
                    for i_ in range(2):
                        psf.append(a2.enter_context(nc.psum_tensor(f"psx{s}_{i_}", [128, 512], F32)))
                        psf_b.append(Buf(f"psx{i_}"))
                    gen_ring[:] = [0, 1, 2, 3, 6, 7]
                    ps_rr[0] = 0
                    NW = 8
                    wring = [sb(f"wring{i}", [128, 16, 128], BF16, a2) for i in range(NW)]
                    wring_b = [Buf(f"wring{i}") for i in range(NW)]
                    wr_rr = [0]

                    def load_chunk(c):
                        i = wr_rr[0] % NW
                        wr_rr[0] += 1
                        Sx.dma("pool", lambda: G.dma_start(out=wring[i][:].rearrange("p k j -> p (k j)").rearrange("p (a n) -> p a n", a=2),
                                                           in_=w_in[c].rearrange("p (a n) -> p a n", a=2)),
                               [], [wring_b[i]], wring_b[i])
                        return wring[i], wring_b[i]

                    def proj_fm(wt, wt_b, tg):
                        pt, pb = bank()
                        mm_group(pt[:], pb, [(wt[:, kc, :], hT[:, kc, tg * 512:(tg + 1) * 512]) for kc in range(16)],
                                 [wt_b] + [b_ for tt_ in range(tg * 4, tg * 4 + 4) for b_ in hT_b[tt_]])
                        return pt, pb

                    with ExitStack() as ar:
                        cs = sb("cs", [128, 2, S], F32, ar); cs_b = Buf("cs")
                        Sx.dma("sp", lambda: SP.dma_start(out=cs[:], in_=cst.rearrange("c p t -> p c t")), [], [cs_b], cs_b)
                        qk = [sb(f"qk{i}", [128, S], BF16, ar) for i in range(4)]
                        qk_b = [Buf(f"qk{i}") for i in range(4)]
                        rt = [sb(f"rt{i}", [128, 512], F32, ar) for i in range(4)]
                        rt_b = [Buf(f"rt{i}") for i in range(4)]
                        v_sb = sb("v_sb", [128, 16, 256], BF16, ar); v_b = Buf("v_sb")
                        gs = [sb(f"gs{i}", [128, S], BF16, ar) for i in range(2)]
                        gs_b = [Buf(f"gs{i}") for i in range(2)]
                        strip = sb("dmk", [128, 4, 512], F32, ar); strip_b = Buf("dmk")
                        cfp = sb("cfp", [128, 2, 512], F32, ar); cfp_b = Buf("cfp")
                        NST = 4
                        sT = [sb(f"sT{i}", [128, 512], BF16, ar) for i in range(NST)]
                        sT_b = [Buf(f"sT{i}") for i in range(NST)]
                        st_rr = [0]
                        ob = sb("ob", [128, 512], BF16, ar); ob_b = Buf("ob")
                        osq = sb("osq", [128, 512], BF16, ar); osq_b = Buf("osq")
                        mean_sb = sb("mean_sb", [128, 512], F32, ar); mean_b = Buf("mean_sb")
                        var_sb = sb("var_sb", [128, 512], F32, ar); var_b = Buf("var_sb")
                        cen = sb("cen", [128, 512], F32, ar); cen_b = Buf("cen")
                        mixrow = sb("mixrow", [128, 2, S], BF16, ar); mixrow_b = Buf("mixrow")

                        pend = [None]

                        def norm_steps(hh, h, qs, po_t, po_b):
                            Sx.op("act", lambda: A.activation(out=ob[:], in_=po_t[:], func=AF.Copy), [po_b], [ob_b])
                            Sx.op("act", lambda: A.activation(out=osq[:], in_=po_t[:], func=AF.Square), [po_b], [osq_b])
                            yield
                            pm, pm_b = bank()
                            mm_group(pm[:], pm_b, [(onesb[:], ob[:])], [onesb_b, ob_b])
                            pq, pq_b = bank()
                            mm_group(pq[:], pq_b, [(onesb[:], osq[:])], [onesb_b, osq_b])
                            Sx.op("act", lambda: A.activation(out=mean_sb[:], in_=pm[:], func=AF.Copy), [pm_b], [mean_b])
                            Sx.op("dve", lambda: V.tensor_tensor(out=var_sb[:], in0=mean_sb[:], in1=mean_sb[:], op=ALU.mult), [mean_b], [var_b])
                            Sx.op("dve", lambda: V.tensor_tensor(out=var_sb[:], in0=pq[:], in1=var_sb[:], op=ALU.subtract), [pq_b, var_b], [var_b])
                            yield
                            Sx.op("dve", lambda: V.tensor_scalar(out=var_sb[:], in0=var_sb[:], scalar1=0.0, scalar2=None, op0=ALU.max), [var_b], [var_b])
                            Sx.op("act", lambda: A.activation(out=var_sb[:], in_=var_sb[:], func=AF.Ln, bias=EPS), [var_b], [var_b])
                            Sx.op("act", lambda: A.activation(out=var_sb[:], in_=var_sb[:], func=AF.Exp, scale=-0.5), [var_b], [var_b])
                            yield
                            Sx.op("dve", lambda: V.tensor_tensor(out=cen[:], in0=po_t[:], in1=mean_sb[:], op=ALU.subtract), [po_b, mean_b], [cen_b])
                            Sx.op("dve", lambda: V.tensor_tensor(out=cen[:], in0=cen[:], in1=var_sb[:], op=ALU.mult), [cen_b, var_b], [cen_b])
                            Sx.op("dve", lambda: V.scalar_tensor_tensor(out=mixrow[:, hh, qs], in0=cen[:], scalar=pv[:, PV_GRET + h:PV_GRET + h + 1],
                                                                        in1=gs[hh][:, qs], op0=ALU.mult, op1=ALU.mult),
                                  [cen_b, pv_b, gs_b[hh]], [mixrow_b])

                        for p in range(4):
                            c0 = p * 8
                            Sx.dma("sp", lambda: SP.dma_start(out=cfp[:], in_=cfq[2 * p:2 * p + 2].rearrange("h p t -> p h t")), [], [cfp_b], cfp_b)
                            for qi in range(2):
                                for hh in range(2):
                                    wa, wa_b = load_chunk(c0 + 2 * qi + hh)
                                    dst, dst_b = qk[2 * qi + hh], qk_b[2 * qi + hh]
                                    for tg in range(4):
                                        pa, pa_b = proj_fm(wa, wa_b, tg)
                                        ts = slice(tg * 512, (tg + 1) * 512)
                                        ri = (tg % 2) * 2
                                        Sx.op("act", lambda: A.activation(out=rt[ri][:], in_=pa[:], func=AF.Copy), [pa_b], [rt_b[ri]])
                                        pp_, pp_b_ = bank()
                                        mm_group(pp_[:], pp_b_, [(permf[:], rt[ri][:])], [permf_b, rt_b[ri]])
                                        Sx.op("dve", lambda: V.tensor_tensor(out=rt[ri + 1][:], in0=pp_[:], in1=cs[:, 1, ts], op=ALU.mult), [pp_b_, cs_b], [rt_b[ri + 1]])
                                        Sx.op("dve", lambda: V.tensor_tensor(out=rt[ri][:], in0=rt[ri][:], in1=cs[:, 0, ts], op=ALU.mult), [rt_b[ri], cs_b], [rt_b[ri]])
                                        if qi == 0:
                                            Sx.op("dve", lambda: V.tensor_tensor(out=rt[ri][:], in0=rt[ri][:], in1=rt[ri + 1][:], op=ALU.add), [rt_b[ri], rt_b[ri + 1]], [rt_b[ri]])
                                            Sx.op("dve", lambda: V.tensor_tensor(out=dst[:, ts], in0=rt[ri][:], in1=cfp[:, hh, :], op=ALU.mult), [rt_b[ri], cfp_b], [dst_b])
                                        else:
                                            Sx.op("dve", lambda: V.tensor_tensor(out=dst[:, ts], in0=rt[ri][:], in1=rt[ri + 1][:], op=ALU.add), [rt_b[ri], rt_b[ri + 1]], [dst_b])
                            wv = [load_chunk(c0 + 4), load_chunk(c0 + 5)]
                            for t2 in range(8):
                                pt, pb = bank()
                                def fn():
                                    last = None
                                    for tt in range(2):
                                        t = t2 * 2 + tt
                                        for hh in range(2):
                                            for kc in range(16):
                                                last = T.matmul(pt[:, tt * 256 + hh * 128: tt * 256 + hh * 128 + 128],
                                                                lhsT=hT[:, kc, t * 128:(t + 1) * 128], rhs=wv[hh][0][:, kc, :],
                                                                start=(kc == 0), stop=(kc == 15))
                                    return last
                                Sx.op("pe", fn, [wv[0][1], wv[1][1]] + hT_b[2 * t2] + hT_b[2 * t2 + 1], [pb])
                                Sx.op("act", lambda: A.activation(out=v_sb[:, 2 * t2:2 * t2 + 2, :], in_=pt[:].rearrange("p (a b) -> p a b", a=2), func=AF.Copy),
                                      [pb], [v_b])
                            for hh in range(2):
                                wgt, wgt_b = load_chunk(c0 + 6 + hh)
                                for tg in range(4):
                                    pt, pb = proj_fm(wgt, wgt_b, tg)
                                    Sx.op("act", lambda: A.activation(out=gs[hh][:, tg * 512:(tg + 1) * 512], in_=pt[:], func=AF.Silu), [pb], [gs_b[hh]])
                            for hh in range(2):
                                h = 2 * p + hh
                                po = hh * 64
                                Sx.dma("sp", lambda: SP.dma_start(out=strip[:], in_=dmask[h].rearrange("p (r t) -> p r t", r=4)), [], [strip_b], strip_b)
                                for g in range(4):
                                    nJ = 4 * g + 4
                                    qs = slice(g * 512, (g + 1) * 512)
                                    po_t, po_b = accbank()

                                    def scores(J):
                                        pt, pb = bank()
                                        ks = slice(J * 128, (J + 1) * 128)
                                        mm_group(pt[:], pb, [(qk[2 + hh][:, ks], qk[hh][:, qs])], [qk_b[hh], qk_b[2 + hh]])
                                        return pt, pb
                                    ahead = [scores(0)]
                                    if nJ > 1:
                                        ahead.append(scores(1))
                                    for J in range(nJ):
                                        cur = ahead.pop(0)
                                        if J + 2 < nJ:
                                            ahead.append(scores(J + 2))
                                        i = st_rr[0] % NST
                                        st_rr[0] += 1
                                        if J < 4 * g:
                                            cc = h * 16 + (4 * g - J)
                                            Sx.op("act", lambda: A.activation(out=sT[i][:], in_=cur[0][:], func=AF.Copy, scale=ctab_sb[:, cc:cc + 1]),
                                                  [cur[1], ctab_b], [sT_b[i]])
                                        else:
                                            Sx.op("dve", lambda: V.tensor_tensor(out=sT[i][:], in0=cur[0][:], in1=strip[:, J - 4 * g, :], op=ALU.mult),
                                                  [cur[1], strip_b], [sT_b[i]])
                                        Sx.op("pe", lambda: T.matmul(po_t[:], lhsT=v_sb[:, J, hh * 128:(hh + 1) * 128], rhs=sT[i][:],
                                                                     start=(J == 0), stop=(J == nJ - 1)),
                                              [v_b, sT_b[i]], [po_b], fifo=True)
                                        if pend[0] is not None:
                                            if next(pend[0], "done") == "done":
                                                pend[0] = None
                                    while pend[0] is not None:
                                        if next(pend[0], "done") == "done":
                                            pend[0] = None
                                    pend[0] = norm_steps(hh, h, qs, po_t, po_b)
                            while pend[0] is not None:
                                if next(pend[0], "done") == "done":
                                    pend[0] = None
                            mb = mixs_b[p % 4]
                            Sx.dma("sp", lambda: SP.dma_start(out=mixT_scr[2 * p:2 * p + 2].rearrange("c p t -> p c t"), in_=mixrow[:]),
                                   [mixrow_b], [mb], mb)
                        Sx.barrier()

                    with ExitStack() as al:
                        ubuf = sb("ubuf", [128, S + 4], F32, al); ubuf_b = Buf("ubuf")
                        uc = sb("uc", [128, S], F32, al); uc_b = Buf("uc")
                        ucb = sb("ucb", [128, S], BF16, al); ucb_b = Buf("ucb")
                        abuf = sb("abuf", [128, S], F32, al); abuf_b = Buf("abuf")
                        ibuf = sb("ibuf", [128, S], F32, al); ibuf_b = Buf("ibuf")
                        hbuf = sb("hbuf", [128, S], F32, al); hbuf_b = Buf("hbuf")
                        gzb = sb("gzb", [128, S], BF16, al); gzb_b = Buf("gzb")
                        hsq = sb("hsq", [128, S], BF16, al); hsq_b = Buf("hsq")
                        mrl = sb("mrl", [128, S], BF16, al); mrl_b = Buf("mrl")
                        zs = sb("zs", [128, 512], F32, al); zs_b = Buf("zs")
                        z2 = sb("z2", [128, 512], F32, al); z2_b = Buf("z2")
                        rs2 = [sb(f"rs{i}", [128, 512], F32, al) for i in range(2)]
                        rs2_b = [Buf(f"rs{i}") for i in range(2)]
                        Sx.op("dve", lambda: V.memset(ubuf[:, 0:3], 0.0), [], [ubuf_b])
                        gzb2 = [gzb, sb("gzb1", [128, S], BF16, al)]
                        gzb2_b = [gzb_b, Buf("gzb1")]

                        def proj_gen(n):
                            wu, wu_b = load_chunk(32 + 2 * n)
                            wz, wz_b = load_chunk(33 + 2 * n)
                            for tg in range(4):
                                pt, pb = proj_fm(wu, wu_b, tg)
                                Sx.op("act", lambda: A.activation(out=ubuf[:, 3 + tg * 512:3 + (tg + 1) * 512], in_=pt[:], func=AF.Copy), [pb], [ubuf_b])
                                yield
                            gz, gz_b = gzb2[n % 2], gzb2_b[n % 2]
                            for tg in range(4):
                                ts = slice(tg * 512, (tg + 1) * 512)
                                pt, pb = proj_fm(wz, wz_b, tg)
                                Sx.op("act", lambda: A.activation(out=zs[:], in_=pt[:], func=AF.Copy), [pb], [zs_b])
                                Sx.op("act", lambda: A.activation(out=z2[:], in_=zs[:], func=AF.Square), [zs_b], [z2_b])
                                yield
                                Sx.op("dve", lambda: V.tensor_scalar(out=z2[:], in0=z2[:], scalar1=0.044715 * GK, scalar2=GK, op0=ALU.mult, op1=ALU.add), [z2_b], [z2_b])
                                Sx.op("dve", lambda: V.tensor_tensor(out=z2[:], in0=z2[:], in1=zs[:], op=ALU.mult), [z2_b, zs_b], [z2_b])
                                Sx.op("act", lambda: A.activation(out=z2[:], in_=z2[:], func=AF.Sigmoid), [z2_b], [z2_b])
                                Sx.op("dve", lambda: V.tensor_tensor(out=gz[:, ts], in0=z2[:], in1=zs[:], op=ALU.mult), [z2_b, zs_b], [gz_b])
                                yield

                        def chain_gen(n):
                            gz, gz_b = gzb2[n % 2], gzb2_b[n % 2]
                            cw = lambda k: pv[:, PV_CW + n * 4 + k:PV_CW + n * 4 + k + 1]
                            Sx.op("dve", lambda: V.tensor_scalar(out=uc[:], in0=ubuf[:, 0:S], scalar1=cw(0), scalar2=pv[:, PV_CB + n:PV_CB + n + 1],
                                                                 op0=ALU.mult, op1=ALU.add), [ubuf_b, pv_b], [uc_b])
                            for k in range(1, 4):
                                Sx.op("dve", lambda: V.scalar_tensor_tensor(out=uc[:], in0=ubuf[:, k:k + S], scalar=cw(k), in1=uc[:], op0=ALU.mult, op1=ALU.add),
                                      [ubuf_b, pv_b, uc_b], [uc_b])
                            yield "ubuf_free"
                            Sx.op("act", lambda: A.activation(out=ucb[:], in_=uc[:], func=AF.Copy), [uc_b], [ucb_b])
                            yield
                            for tg in range(4):
                                ts = slice(tg * 512, (tg + 1) * 512)
                                pt, pb = bank()
                                mm_group(pt[:], pb, [(wg_sb[:, n, :], ucb[:, ts])], [wg_b, ucb_b])
                                Sx.op("act", lambda: A.activation(out=abuf[:, ts], in_=pt[:], func=AF.Sigmoid, bias=pv[:, PV_BRG + n:PV_BRG + n + 1]), [pb, pv_b], [abuf_b])
                                pt2, pb2 = bank()
                                mm_group(pt2[:], pb2, [(wg_sb[:, 8 + n, :], ucb[:, ts])], [wg_b, ucb_b])
                                Sx.op("act", lambda: A.activation(out=ibuf[:, ts], in_=pt2[:], func=AF.Sigmoid, bias=pv[:, PV_BIG + n:PV_BIG + n + 1]), [pb2, pv_b], [ibuf_b])
                                yield
                            Sx.op("act", lambda: A.activation(out=abuf[:], in_=abuf[:], func=AF.Exp, scale=scl[:, n:n + 1]), [abuf_b, scl_b], [abuf_b])
                            yield
                            Sx.op("dve", lambda: V.tensor_tensor(out=hbuf[:], in0=abuf[:], in1=abuf[:], op=ALU.mult), [abuf_b], [hbuf_b])
                            Sx.op("dve", lambda: V.tensor_scalar(out=hbuf[:], in0=hbuf[:], scalar1=-1.0, scalar2=1.0, op0=ALU.mult, op1=ALU.add), [hbuf_b], [hbuf_b])
                            yield
                            Sx.op("act", lambda: A.activation(out=hbuf[:], in_=hbuf[:], func=AF.Sqrt), [hbuf_b], [hbuf_b])
                            yield
                            Sx.op("dve", lambda: V.tensor_tensor(out=ibuf[:], in0=ibuf[:], in1=hbuf[:], op=ALU.mult), [ibuf_b, hbuf_b], [ibuf_b])
                            yield
                            Sx.op("dve", lambda: V.tensor_tensor(out=ibuf[:], in0=ibuf[:], in1=uc[:], op=ALU.mult), [ibuf_b, uc_b], [ibuf_b])
                            yield
                            Sx.op("dve", lambda: V.tensor_tensor_scan(out=hbuf[:], data0=abuf[:], data1=ibuf[:], initial=0.0, op0=ALU.mult, op1=ALU.add),
                                  [abuf_b, ibuf_b], [hbuf_b])
                            yield
                            Sx.op("act", lambda: A.activation(out=hsq[:], in_=hbuf[:], func=AF.Square), [hbuf_b], [hsq_b])
                            yield
                            for tg in range(4):
                                ts = slice(tg * 512, (tg + 1) * 512)
                                pt, pb = bank()
                                rs, rs_b = rs2[tg % 2], rs2_b[tg % 2]
                                mm_group(pt[:], pb, [(onesb[:], hsq[:, ts])], [onesb_b, hsq_b])
                                Sx.op("act", lambda: A.activation(out=rs[:], in_=pt[:], func=AF.Ln, bias=EPS), [pb], [rs_b])
                                Sx.op("act", lambda: A.activation(out=rs[:], in_=rs[:], func=AF.Exp, scale=-0.5), [rs_b], [rs_b])
                                Sx.op("dve", lambda: V.tensor_tensor(out=rs[:], in0=rs[:], in1=hbuf[:, ts], op=ALU.mult), [rs_b, hbuf_b], [rs_b])
                                Sx.op("dve", lambda: V.scalar_tensor_tensor(out=mrl[:, ts], in0=rs[:], scalar=pv[:, PV_GLRU + n:PV_GLRU + n + 1], in1=gz[:, ts],
                                                                            op0=ALU.mult, op1=ALU.mult), [rs_b, pv_b, gz_b], [mrl_b])
                                yield
                            mb = mixs_b[n % 4]
                            Sx.dma("sp", lambda: SP.dma_start(out=mixT_scr[8 + n], in_=mrl[:]), [mrl_b], [mb], mb)

                        pg0 = proj_gen(0)
                        for _ in pg0:
                            pass
                        for n in range(8):
                            cg = chain_gen(n)
                            while next(cg, "done") != "ubuf_free":
                                pass
                            pg = proj_gen(n + 1) if n + 1 < 8 else None
                            cdone, pdone = False, pg is None
                            while not (cdone and pdone):
                                if not cdone and next(cg, "done") == "done":
                                    cdone = True
                                if not pdone and next(pg, "done") == "done":
                                    pdone = True
                        Sx.barrier()

            del psf[6:], psf_b[6:]
            gen_ring[:] = [0, 1, 2, 3]
            ps_rr[0] = 0
            if debug == "mix" and s == 0:
                db_ = Buf("dbgmix")
                Sx.dma("sp", lambda: SP.dma_start(out=dbg["mix"], in_=mixT_scr), mixs_b, [db_], db_)
                Sx.barrier()
                stop[0] = True
                break
            with ExitStack() as pb_:
                wo = sb("wo", [128, 16, D], BF16, pb_); wo_b = [Buf(f"wo{i}") for i in range(4)]
                wov = w_out.rearrange("(kc p) n -> p kc n", p=128)
                for i in range(4):
                    Sx.dma("pool", lambda: [G.dma_start(out=wo[:, kc, :].rearrange("p (a n) -> p a n", a=2), in_=wov[:, kc, :].rearrange("p (a n) -> p a n", a=2))
                                            for kc in range(4 * i, 4 * i + 4)], [], [wo_b[i]], wo_b[i])
                mixg = [sb(f"mixg{i}", [128, 16, 512], BF16, pb_) for i in range(2)]
                mixg_b = [Buf(f"mixg{i}") for i in range(2)]
                xin = [sb(f"xinb{i}", [128, D], F32, pb_) for i in range(2)]
                xin_b = [Buf(f"xinb{i}") for i in range(2)]
                xm = [sb(f"xm{i}", [128, D], F32, pb_) for i in range(2)]
                xm_b = [Buf(f"xm{i}") for i in range(2)]
                h2b = [sb(f"h2b{i}", [128, D], BF16, pb_) for i in range(2)]
                h2b_b = [Buf(f"h2b{i}") for i in range(2)]
                gfb = sb("gfb", [128, D], F32, pb_); gfb_b = Buf("gfb")
                junk = sb("junkb", [128, D], BF16, pb_); junk_b = Buf("junkb")
                xmT2 = [sb(f"xmT{i}", [128, 16, 128], F32, pb_) for i in range(2)]
                xmT2_b = [[Buf(f"xmT{i}_{k}") for k in range(16)] for i in range(2)]
                NSM = 24
                sm = sb("sm", [128, NSM, 40], F32, pb_)
                sm_b = [Buf(f"sm{i}") for i in range(NSM)]
                m8 = sb("m8", [128, 8], F32, pb_); m8_b = Buf("m8")
                i8 = sb("i8", [128, 8], U32, pb_); i8_b = Buf("i8")
                Mb = sb("Mb", [128, 32], BF16, pb_); Mb_b = Buf("Mb")
                sidx = sb("sidx", [128, 2], I32, pb_); sidx_b = Buf("sidx")
                pk = [sb(f"pk{i}", [128, 2], F32, pb_) for i in range(2)]
                pk_b = [Buf(f"pk{i}") for i in range(2)]
                stab_w = Buf("slot_tab_w")
                Sx.dma("sp", lambda: SP.dma_start(out=gfb[:], in_=gffn), [], [gfb_b], gfb_b)
                mixv = mixT_scr.rearrange("c p t -> p c t")
                def XB(t, ms):
                    tt = t % 4
                    gt = s * 16 + t
                    sl = t % 2
                    r0 = s * S + t * 128
                    if t == 0:
                        Sx.dma("sp", lambda: SP.dma_start(out=xin[0][:], in_=x[s * S:s * S + 128, :]), [], [xin_b[0]], xin_b[0])
                    if t + 1 < 16:
                        r1_ = s * S + (t + 1) * 128
                        Sx.dma("sp", lambda: SP.dma_start(out=xin[(t + 1) % 2][:], in_=x[r1_:r1_ + 128, :]), [], [xin_b[(t + 1) % 2]], xin_b[(t + 1) % 2])
                    for cg in range(4):
                        pt, pbk = bank()
                        cs_ = slice(cg * 512, (cg + 1) * 512)
                        mm_group(pt[:], pbk, [(mixg[ms][:, kc, tt * 128:(tt + 1) * 128], wo[:, kc, cs_]) for kc in range(16)],
                                 [mixg_b[ms]] + wo_b)
                        Sx.op("dve", lambda: V.tensor_tensor(out=xm[sl][:, cs_], in0=pt[:], in1=xin[sl][:, cs_], op=ALU.add),
                              [pbk, xin_b[sl]], [xm_b[sl]])
                    xb = xmid_b[t % 2]
                    Sx.dma("sp", lambda: SP.dma_start(out=xmid_scr[r0:r0 + 128, :], in_=xm[sl][:]), [xm_b[sl]], [xb], xb)
                    ss = sm[:, 19 + t % 4, 0:1]
                    Sx.op("act", lambda: A.activation(out=junk[:], in_=xm[sl][:], func=AF.Square, accum_out=ss), [xm_b[sl]], [junk_b, sm_b[19 + t % 4]])
                    rstd_from_ss(ss, sm_b[19 + t % 4], D)
                    Sx.op("dve", lambda: V.scalar_tensor_tensor(out=h2b[sl][:], in0=xm[sl][:], scalar=ss, in1=gfb[:], op0=ALU.mult, op1=ALU.mult),
                          [xm_b[sl], sm_b[19 + t % 4], gfb_b], [h2b_b[sl]])
                    hb_ = h2s_b[t % 2]
                    Sx.dma("sp", lambda: SP.dma_start(out=h2_scr[r0:r0 + 128, :], in_=h2b[sl][:]), [h2b_b[sl]], [hb_], hb_)

                def RB(t):
                    gt = s * 16 + t
                    sl = t % 2
                    xmT, xmT_b = xmT2[t % 2], xmT2_b[t % 2]
                    ss = sm[:, 19 + t % 4, 0:1]
                    for q in range(4):
                        pt, pbk = bank()
                        def fn():
                            last = None
                            for j in range(4):
                                kc = q * 4 + j
                                last = T.transpose(out=pt[:, j * 128:(j + 1) * 128], in_=xm[sl][:, kc * 128:(kc + 1) * 128], identity=identf[:])
                            return last
                        Sx.op("pe", fn, [xm_b[sl], identf_b], [pbk])
                        for j in range(4):
                            kc = q * 4 + j
                            if q % 2 == 0:
                                Sx.op("dve", lambda: V.tensor_scalar(out=xmT[:, kc, :], in0=pt[:, j * 128:(j + 1) * 128],
                                                                     scalar1=pv[:, PV_GFFN + kc:PV_GFFN + kc + 1], scalar2=None, op0=ALU.mult),
                                      [pbk, pv_b], [xmT_b[kc]])
                            else:
                                Sx.op("act", lambda: A.activation(out=xmT[:, kc, :], in_=pt[:, j * 128:(j + 1) * 128], func=AF.Copy,
                                                                  scale=pv[:, PV_GFFN + kc:PV_GFFN + kc + 1]), [pbk, pv_b], [xmT_b[kc]])
                    yield
                    pl, pl_b = bank()
                    mm_group(pl[:, 0:36], pl_b, [(xmT[:, kc, :], wrs[:, kc, :]) for kc in range(16)], xmT_b + [wrs_b])
                    lg = sm[:, 1, 0:36]
                    Sx.op("dve", lambda: V.scalar_tensor_tensor(out=lg, in0=pl[:, 0:36], scalar=ss, in1=brs[:], op0=ALU.mult, op1=ALU.add),
                          [pl_b, sm_b[19 + t % 4], brs_b], [sm_b[1]])
                    gmax = sm[:, 2, 0:1]
                    Sx.op("dve", lambda: V.tensor_reduce(out=gmax, in_=sm[:, 1, 0:4], axis=AX.X, op=ALU.max), [sm_b[1]], [sm_b[2]])
                    ngmax = sm[:, 3, 0:1]
                    Sx.op("dve", lambda: V.tensor_scalar(out=ngmax, in0=gmax, scalar1=-1.0, scalar2=None, op0=ALU.mult), [sm_b[2]], [sm_b[3]])
                    gexp = sm[:, 4, 0:4]
                    Sx.op("act", lambda: A.activation(out=gexp, in_=sm[:, 1, 0:4], func=AF.Exp, bias=ngmax), [sm_b[1], sm_b[3]], [sm_b[4]])
                    gp = sm[:, 5, 0:1]
                    Sx.op("dve", lambda: V.tensor_reduce(out=gp, in_=gexp, axis=AX.X, op=ALU.add), [sm_b[4]], [sm_b[5]])
                    Sx.op("dve", lambda: V.reciprocal(out=gp, in_=gp), [sm_b[5]], [sm_b[5]])
                    pen = sm[:, 6, 0:4]
                    Sx.op("dve", lambda: V.tensor_scalar(out=pen, in0=sm[:, 1, 0:4], scalar1=gmax, scalar2=None, op0=ALU.is_ge), [sm_b[1], sm_b[2]], [sm_b[6]])
                    Sx.op("dve", lambda: V.tensor_scalar(out=pen, in0=pen, scalar1=1e9, scalar2=-1e9, op0=ALU.mult, op1=ALU.add), [sm_b[6]], [sm_b[6]])
                    el = sm[:, 7, 0:32]
                    for g4 in range(4):
                        Sx.op("dve", lambda: V.tensor_scalar(out=sm[:, 7, g4 * 8:(g4 + 1) * 8], in0=sm[:, 1, 4 + g4 * 8:4 + (g4 + 1) * 8],
                                                             scalar1=sm[:, 6, g4:g4 + 1], scalar2=None, op0=ALU.add), [sm_b[1], sm_b[6]], [sm_b[7]])
                    Sx.op("dve", lambda: V.max(out=m8[:], in_=el), [sm_b[7]], [m8_b])
                    Sx.op("dve", lambda: V.max_index(out=i8[:], in_max=m8[:], in_values=el), [sm_b[7], m8_b], [i8_b])
                    ef = sm[:, 8, 0:2]
                    Sx.op("dve", lambda: V.tensor_copy(out=ef, in_=i8[:, 0:2]), [i8_b], [sm_b[8]])
                    ed = sm[:, 9, 0:1]
                    Sx.op("dve", lambda: V.tensor_tensor(out=ed, in0=m8[:, 1:2], in1=m8[:, 0:1], op=ALU.subtract), [m8_b], [sm_b[9]])
                    Sx.op("act", lambda: A.activation(out=ed, in_=ed, func=AF.Exp), [sm_b[9]], [sm_b[9]])
                    w12 = sm[:, 10, 0:2]
                    Sx.op("dve", lambda: V.tensor_scalar(out=sm[:, 10, 0:1], in0=ed, scalar1=1.0, scalar2=None, op0=ALU.add), [sm_b[9]], [sm_b[10]])
                    Sx.op("dve", lambda: V.reciprocal(out=sm[:, 10, 0:1], in_=sm[:, 10, 0:1]), [sm_b[10]], [sm_b[10]])
                    Sx.op("dve", lambda: V.tensor_tensor(out=sm[:, 10, 1:2], in0=sm[:, 10, 0:1], in1=ed, op=ALU.mult), [sm_b[10], sm_b[9]], [sm_b[10]])
                    Sx.op("dve", lambda: V.tensor_scalar(out=w12, in0=w12, scalar1=gp, scalar2=None, op0=ALU.mult), [sm_b[10], sm_b[5]], [sm_b[10]])
                    M1, M2 = sm[:, 11, 0:32], sm[:, 12, 0:32]
                    Sx.op("dve", lambda: V.tensor_scalar(out=M1, in0=iota_e[:], scalar1=sm[:, 8, 0:1], scalar2=None, op0=ALU.is_equal), [iota_e_b, sm_b[8]], [sm_b[11]])
                    Sx.op("dve", lambda: V.tensor_scalar(out=M2, in0=iota_e[:], scalar1=sm[:, 8, 1:2], scalar2=None, op0=ALU.is_equal), [iota_e_b, sm_b[8]], [sm_b[12]])
                    Sx.op("dve", lambda: V.tensor_tensor(out=Mb[:], in0=M1, in1=M2, op=ALU.add), [sm_b[11], sm_b[12]], [Mb_b])
                    yield
                    pp, pp_b = bank()
                    mm_group(pp[:, 0:32], pp_b, [(ustr[:], Mb[:]), (onefull[:], Rb[:])], [ustr_b, Mb_b, onefull_b, Rb_b])
                    pos = sm[:, 13, 0:32]
                    Sx.op("act", lambda: A.activation(out=pos, in_=pp[:, 0:32], func=AF.Copy), [pp_b], [sm_b[13]])
                    Sx.op("dve", lambda: V.tensor_tensor(out=Rb[:], in0=Rb[:], in1=Mb[:], op=ALU.add), [Rb_b, Mb_b], [Rb_b])
                    p12 = sm[:, 14, 0:2]
                    for k, Mk in enumerate((M1, M2)):
                        Sx.op("dve", lambda: V.tensor_tensor(out=sm[:, 15 + k, 0:32], in0=Mk, in1=pos, op=ALU.mult), [sm_b[11 + k], sm_b[13]], [sm_b[15 + k]])
                        Sx.op("dve", lambda: V.tensor_reduce(out=sm[:, 14, k:k + 1], in_=sm[:, 15 + k, 0:32], axis=AX.X, op=ALU.add), [sm_b[15 + k]], [sm_b[14]])
                    slot = sm[:, 17, 0:2]
                    Sx.op("dve", lambda: V.scalar_tensor_tensor(out=slot, in0=ef, scalar=float(CAP), in1=p12, op0=ALU.mult, op1=ALU.add), [sm_b[8], sm_b[14]], [sm_b[17]])
                    vld = sm[:, 18, 0:2]
                    Sx.op("dve", lambda: V.tensor_single_scalar(out=vld, in_=p12, scalar=float(CAP), op=ALU.is_lt), [sm_b[14]], [sm_b[18]])
                    Sx.op("dve", lambda: V.tensor_scalar(out=slot, in0=slot, scalar1=dummyp[:, 0:1], scalar2=None, op0=ALU.subtract), [sm_b[17], dummyp_b], [sm_b[17]])
                    Sx.op("dve", lambda: V.tensor_tensor(out=slot, in0=slot, in1=vld, op=ALU.mult), [sm_b[17], sm_b[18]], [sm_b[17]])
                    Sx.op("dve", lambda: V.tensor_scalar(out=slot, in0=slot, scalar1=dummyp[:, 0:1], scalar2=None, op0=ALU.add), [sm_b[17], dummyp_b], [sm_b[17]])
                    Sx.op("dve", lambda: V.tensor_copy(out=sidx[:], in_=slot), [sm_b[17]], [sidx_b])
                    Sx.op("dve", lambda: V.tensor_copy(out=gix[:, gt, :], in_=slot), [sm_b[17]], [gix_b])
                    for k in range(2):
                        Sx.op("dve", lambda: V.tensor_copy(out=pk[k][:, 0:1], in_=tokid[:, gt:gt + 1]), [tokid_b], [pk_b[k]])
                        Sx.op("dve", lambda: V.tensor_copy(out=pk[k][:, 1:2], in_=sm[:, 10, k:k + 1]), [sm_b[10], pk_b[k]], [pk_b[k]])
                        Sx.dma("pool", lambda: G.indirect_dma_start(out=slot_tab, out_offset=bass.IndirectOffsetOnAxis(ap=sidx[:, k:k + 1], axis=0),
                                                                    in_=pk[k][:], in_offset=None),
                               [pk_b[k], sidx_b], [stab_w], stab_w)

                pend_r = []
                for tg in range(4):
                    ms = tg % 2
                    Sx.dma("sp", lambda: SP.dma_start(out=mixg[ms][:], in_=mixv[:, :, tg * 512:(tg + 1) * 512]), mixs_b, [mixg_b[ms]], mixg_b[ms])
                    for tt in range(4):
                        t = tg * 4 + tt
                        XB(t, ms)
                        order_ = ([pend_r[-1]] + pend_r[:-1]) if pend_r else []
                        for g_ in order_:
                            if next(g_, "done") == "done":
                                pend_r.remove(g_)
                        pend_r.append(RB(t))
                for g_ in pend_r:
                    while next(g_, "done") != "done":
                        pass
                Sx.barrier()

        if debug == "xmid":
            db_ = Buf("dbgx")
            Sx.dma("sp", lambda: [SP.dma_start(out=dbg["xmid"], in_=xmid_scr), SP.dma_start(out=dbg["h2"], in_=h2_scr),
                                  SP.dma_start(out=dbg["stab"], in_=slot_tab),
                                  SP.dma_start(out=dbg["gix"], in_=gix[:].rearrange("p a b -> p (a b)"))], [gix_b], [db_], db_)
            Sx.barrier()
            stop[0] = True
        NJ = CAP // 128
        with ExitStack() as pc:
          if not stop[0]:
            NR = 6
            psb = [pc.enter_context(nc.psum_tensor(f"psb{i}", [128, 1024], BF16)) for i in range(2)]
            psb_b = [Buf(f"psb{i}") for i in range(2)]
            ring = [sb(f"ring{i}", [128, 8192], BF16, pc) for i in range(NR)]
            ring_b = [Buf(f"ring{i}") for i in range(NR)]
            rr = [0]
            stt = [sb(f"stt{i}", [128, NJ, 2], F32, pc) for i in range(2)]
            stt_b = [Buf(f"stt{i}") for i in range(2)]
            tix = [sb(f"tix{i}", [128, NJ], I32, pc) for i in range(2)]
            tix_b = [Buf(f"tix{i}") for i in range(2)]
            xg = [[sb(f"xg{i}_{j}", [128, D], BF16, pc) for j in range(NJ)] for i in range(2)]
            xg_b = [[Buf(f"xg{i}_{j}") for j in range(NJ)] for i in range(2)]
            XT = [sb(f"XT{i}", [128, 16, CAP], BF16, pc) for i in range(2)]
            XT_b = [Buf(f"XT{i}") for i in range(2)]
            HT = [sb(f"HT{i}", [128, 8, CAP], BF16, pc) for i in range(2)]
            HT_b = [Buf(f"HT{i}") for i in range(2)]
            sgt = [sb(f"sgt{i}", [128, CAP], F32, pc) for i in range(2)]
            sgt_b = [Buf(f"sgt{i}") for i in range(2)]
            yst = [sb(f"yst{i}", [128, D], F32, pc) for i in range(2)]
            yst_b = [Buf(f"yst{i}") for i in range(2)]
            yr = [0]
            stab_r = Buf("slot_tab_r")

            def prep(e):
                b = e % 2
                Sx.dma("sp", lambda: SP.dma_start(out=stt[b][:], in_=slot_tab[e * CAP:(e + 1) * CAP, :].rearrange("(j p) c -> p j c", p=128)),
                       [], [stt_b[b]], stt_b[b])
                Sx.op("dve", lambda: V.tensor_copy(out=tix[b][:], in_=stt[b][:, :, 0]), [stt_b[b]], [tix_b[b]])
                for j in range(NJ):
                    Sx.dma("pool", lambda: G.indirect_dma_start(out=xg[b][j][:], out_offset=None, in_=h2_scr,
                                                                in_offset=bass.IndirectOffsetOnAxis(ap=tix[b][:, j:j + 1], axis=0)),
                           [tix_b[b]], [xg_b[b][j]], xg_b[b][j])

            def rv(i, k):
                return ring[i][:].rearrange("p (k n) -> p k n", k=k)

            def load_gu(e, half):
                for base, src in ((0, w_gate), (2, w_up)):
                    i = base + half
                    v = src[e].rearrange("(kc p) n -> p kc n", p=128)
                    Sx.dma("pool", lambda: G.dma_start(out=rv(i, 16), in_=v[:, :, half * 512:(half + 1) * 512]),
                           [], [ring_b[i]], ring_b[i])

            def load_d(e, half):
                i = 4 + half
                v = w_down[e].rearrange("(kc p) n -> p kc n", p=128)
                Sx.dma("pool", lambda: [G.dma_start(out=ring[i][:, k4 * 2048:(k4 + 1) * 2048].rearrange("p (a n) -> p a n", a=2),
                                                    in_=v[:, half * 4 + k4, :].rearrange("p (a n) -> p a n", a=2)) for k4 in range(4)],
                       [], [ring_b[i]], ring_b[i])

            prep(0)
            load_gu(0, 0); load_gu(0, 1); load_d(0, 0); load_d(0, 1)
            g_w = [(rv(0, 16), ring_b[0]), (rv(1, 16), ring_b[1])]
            u_w = [(rv(2, 16), ring_b[2]), (rv(3, 16), ring_b[3])]
            d_w = [(rv(4, 4), ring_b[4]), (rv(5, 4), ring_b[5])]
            for e in range(NE):
                b = e % 2
                if e + 1 < NE:
                    prep(e + 1)
                for j in range(NJ):
                    for hf in range(2):
                        pt, pbk = psb[hf], psb_b[hf]
                        def fn():
                            last = None
                            for q in range(8):
                                kc = hf * 8 + q
                                last = T.transpose(out=pt[:, q * 128:(q + 1) * 128], in_=xg[b][j][:, kc * 128:(kc + 1) * 128], identity=identb[:])
                            return last
                        Sx.op("pe", fn, [xg_b[b][j], identb_b], [pbk])
                        eng = "act" if hf == 0 else "dve"
                        if eng == "act":
                            Sx.op("act", lambda: A.activation(out=XT[b][:, hf * 8:(hf + 1) * 8, j * 128:(j + 1) * 128],
                                                              in_=pt[:].rearrange("p (k t) -> p k t", k=8), func=AF.Copy), [pbk], [XT_b[b]])
                        else:
                            Sx.op("dve", lambda: V.tensor_copy(out=XT[b][:, hf * 8:(hf + 1) * 8, j * 128:(j + 1) * 128],
                                                               in_=pt[:].rearrange("p (k t) -> p k t", k=8)), [pbk], [XT_b[b]])
                for mc in range(8):
                    half, ml = mc // 4, mc % 4
                    pg, pg_b = bank()
                    mm_group(pg[:, 0:CAP], pg_b, [(g_w[half][0][:, kc, ml * 128:(ml + 1) * 128], XT[b][:, kc, :]) for kc in range(16)],
                             [g_w[half][1], XT_b[b]])
                    pu, pu_b = bank()
                    mm_group(pu[:, 0:CAP], pu_b, [(u_w[half][0][:, kc, ml * 128:(ml + 1) * 128], XT[b][:, kc, :]) for kc in range(16)],
                             [u_w[half][1], XT_b[b]])
                    si = mc % 2
                    Sx.op("act", lambda: A.activation(out=sgt[si][:], in_=pg[:, 0:CAP], func=AF.Silu), [pg_b], [sgt_b[si]])
                    Sx.op("dve", lambda: V.tensor_tensor(out=HT[b][:, mc, :], in0=pu[:, 0:CAP], in1=sgt[si][:], op=ALU.mult), [pu_b, sgt_b[si]], [HT_b[b]])
                    if ml == 3 and e + 1 < NE:
                        load_gu(e + 1, half)
                for j in range(NJ):
                    yi = yr[0] % 2
                    yr[0] += 1
                    for cg in range(4):
                        pt, pbk = bank()
                        cs_ = slice(cg * 512, (cg + 1) * 512)
                        mm_group(pt[:], pbk, [(HT[b][:, mc, j * 128:(j + 1) * 128], d_w[mc // 4][0][:, mc % 4, cs_]) for mc in range(8)],
                                 [HT_b[b], d_w[0][1], d_w[1][1]])
                        if cg % 2 == 0:
                            Sx.op("act", lambda: A.activation(out=yst[yi][:, cs_], in_=pt[:], func=AF.Copy, scale=stt[b][:, j, 1:2]), [pbk, stt_b[b]], [yst_b[yi]])
                        else:
                            Sx.op("dve", lambda: V.tensor_scalar(out=yst[yi][:, cs_], in0=pt[:], scalar1=stt[b][:, j, 1:2], scalar2=None, op0=ALU.mult),
                                  [pbk, stt_b[b]], [yst_b[yi]])
                    r0 = e * CAP + j * 128
                    yb = ysl_b[yi]
                    Sx.dma("sp", lambda: SP.dma_start(out=yslot_scr[r0:r0 + 128, :], in_=yst[yi][:]), [yst_b[yi]], [yb], yb)
                if e + 1 < NE:
                    load_d(e + 1, 0); load_d(e + 1, 1)
            Sx.barrier()

        if debug == "moe":
            db_ = Buf("dbgy")
            Sx.dma("sp", lambda: SP.dma_start(out=dbg["ys"], in_=yslot_scr), [], [db_], db_)
            Sx.barrier()
            stop[0] = True
        with ExitStack() as pd:
          if not stop[0]:
            xf = [sb(f"xf{i}", [128, D], F32, pd) for i in range(3)]
            xf_b = [Buf(f"xf{i}") for i in range(3)]
            y1 = [sb(f"y1{i}", [128, D], F32, pd) for i in range(3)]
            y1_b = [Buf(f"y1{i}") for i in range(3)]
            y2 = [sb(f"y2{i}", [128, D], F32, pd) for i in range(3)]
            y2_b = [Buf(f"y2{i}") for i in range(3)]
            ot = [sb(f"ot{i}", [128, D], F32, pd) for i in range(3)]
            ot_b = [Buf(f"ot{i}") for i in range(3)]
            gfn = sb("gfn", [128, D], F32, pd); gfn_b = Buf("gfn")
            junk = sb("junkd", [128, D], BF16, pd); junk_b = Buf("junkd")
            ssd = [sb(f"ssd{i}", [128, 1], F32, pd) for i in range(3)]
            ssd_b = [Buf(f"ssd{i}") for i in range(3)]
            out_b = [Buf(f"out{i}") for i in range(3)]
            Sx.dma("sp", lambda: SP.dma_start(out=gfn[:], in_=gfin), [], [gfn_b], gfn_b)
            def d_loads(gt):
                sl = gt % 3
                r0 = gt * 128
                Sx.dma("sp", lambda: SP.dma_start(out=xf[sl][:], in_=xmid_scr[r0:r0 + 128, :]), [], [xf_b[sl]], xf_b[sl])
                Sx.dma("pool", lambda: G.indirect_dma_start(out=y1[sl][:], out_offset=None, in_=yslot_scr,
                                                            in_offset=bass.IndirectOffsetOnAxis(ap=gix[:, gt, 0:1], axis=0)),
                       [gix_b], [y1_b[sl]], y1_b[sl])
                Sx.dma("pool", lambda: G.indirect_dma_start(out=y2[sl][:], out_offset=None, in_=yslot_scr,
                                                            in_offset=bass.IndirectOffsetOnAxis(ap=gix[:, gt, 1:2], axis=0)),
                       [gix_b], [y2_b[sl]], y2_b[sl])

            def d_compute(gt):
                sl = gt % 3
                r0 = gt * 128
                Sx.op("dve", lambda: V.tensor_tensor(out=xf[sl][:], in0=xf[sl][:], in1=y1[sl][:], op=ALU.add), [xf_b[sl], y1_b[sl]], [xf_b[sl]])
                Sx.op("dve", lambda: V.tensor_tensor(out=xf[sl][:], in0=xf[sl][:], in1=y2[sl][:], op=ALU.add), [xf_b[sl], y2_b[sl]], [xf_b[sl]])
                Sx.op("act", lambda: A.activation(out=junk[:], in_=xf[sl][:], func=AF.Square, accum_out=ssd[sl][:]), [xf_b[sl]], [junk_b, ssd_b[sl]])
                rstd_from_ss(ssd[sl][:], ssd_b[sl], D)
                Sx.op("dve", lambda: V.scalar_tensor_tensor(out=ot[sl][:], in0=xf[sl][:], scalar=ssd[sl][:, 0:1], in1=gfn[:], op0=ALU.mult, op1=ALU.mult),
                      [xf_b[sl], ssd_b[sl], gfn_b], [ot_b[sl]])
                Sx.dma("sp", lambda: SP.dma_start(out=out[r0:r0 + 128, :], in_=ot[sl][:]), [ot_b[sl]], [out_b[sl]], out_b[sl])

            d_loads(0)
            d_loads(1)
            for gt in range(32):
                if gt + 2 < 32:
                    d_loads(gt + 2)
                d_compute(gt)
            Sx.barrier()
    return nc


def _const_tables():
    half = 64
    inv = (10000.0 ** (-np.arange(half, dtype=np.float32) / half)).astype(np.float32)
    ang = np.arange(S, dtype=np.float32)[None, :] * inv[:, None]
    cos = np.cos(ang).astype(np.float32)
    sin = np.sin(ang).astype(np.float32)
    cst = np.stack([np.concatenate([cos, cos], 0), np.concatenate([-sin, sin], 0)], 0)
    hh = np.arange(8, dtype=np.float64)
    log_g = np.log1p(-(2.0 ** (-5.0 - hh)))
    sc = 128.0 ** -0.5
    jj = np.arange(128, dtype=np.float64)
    ii = np.arange(512, dtype=np.float64)
    cf = np.exp(log_g[:, None] * ii[None, :])
    cfq = np.broadcast_to(cf[:, None, :], (8, 128, 512))
    dl = np.arange(16, dtype=np.float64)
    ct = np.exp(log_g[:, None, None] * (128.0 * dl[None, None, :] - jj[None, :, None])) * sc
    ctab = np.zeros((128, 128), np.float64)
    for h_ in range(8):
        ctab[:, h_ * 16:(h_ + 1) * 16] = ct[h_]
    rr_ = np.arange(4, dtype=np.float64)
    thr = 128.0 * rr_[None, :, None] + jj[:, None, None]
    valid = ii[None, None, :] >= thr
    dm = np.where(valid[None], np.exp(-log_g[:, None, None, None] * thr[None]) * sc, 0.0)
    strips = (cfq, ctab, dm.reshape(8, 128, 2048))
    return np.ascontiguousarray(cst, dtype=np.float32), tuple(np.ascontiguousarray(a_, dtype=np.float32) for a_ in strips)


def _w_in_cols():
    R = 1024
    cols = []
    for p in range(4):
        h0, h1 = 2 * p, 2 * p + 1
        for base in (0, R):
            for h in (h0, h1):
                cols += list(range(base + h * 128, base + h * 128 + 128))
        cols += list(range(2 * R + h0 * 128, 2 * R + h0 * 128 + 256))
        cols += list(range(3 * R + h0 * 128, 3 * R + h0 * 128 + 128))
        cols += list(range(3 * R + h1 * 128, 3 * R + h1 * 128 + 128))
    for n in range(8):
        cols += list(range(4 * R + n * 128, 4 * R + n * 128 + 128))
        cols += list(range(5 * R + n * 128, 5 * R + n * 128 + 128))
    return np.array(cols)


_NC_CACHE = {}


def kernel(x, norm_mix_g, w_in, ret_norm_g, conv_w, conv_b, w_rg, b_rg, w_ig, b_ig,
           lru_lambda, lru_norm_g, w_out, norm_ffn_g, w_group, b_group, w_router, b_router,
           w_gate, w_up, w_down, norm_final_g):
    f = lambda a: np.ascontiguousarray(np.asarray(a), dtype=np.float32)
    x = f(x).reshape(NCORES, TPC, D)
    wi = f(w_in)[0][:, _w_in_cols()]
    wi = np.ascontiguousarray(wi.reshape(16, 128, 48, 128).transpose(2, 1, 0, 3)).reshape(48, 128, 2048)
    pvec = np.zeros((128, 128), np.float32)
    pvec[:, 0:16] = f(norm_mix_g)[0].reshape(16, 128).T
    pvec[:, 16:32] = f(norm_ffn_g)[0].reshape(16, 128).T
    pvec[:, 32:40] = f(ret_norm_g)[0].reshape(8, 128).T
    cw = f(conv_w)[0]
    for n in range(8):
        for k in range(4):
            pvec[:, 40 + n * 4 + k] = cw[k, n * 128:(n + 1) * 128]
    pvec[:, 72:80] = f(conv_b)[0].reshape(8, 128).T
    pvec[:, 80:88] = f(b_rg)[0].reshape(8, 128).T
    pvec[:, 88:96] = f(b_ig)[0].reshape(8, 128).T
    pvec[:, 96:104] = f(lru_lambda)[0].reshape(8, 128).T
    pvec[:, 104:112] = f(lru_norm_g)[0].reshape(8, 128).T
    wgates = np.ascontiguousarray(np.concatenate([f(w_rg)[0], f(w_ig)[0]], 0))
    wr = np.ascontiguousarray(np.concatenate([f(w_group)[0], f(w_router)[0]], 1))
    brow = np.ascontiguousarray(np.broadcast_to(np.concatenate([f(b_group)[0], f(b_router)[0]])[None, :], (128, 36)))
    gfin = np.ascontiguousarray(np.broadcast_to(f(norm_final_g)[None, :], (128, D)))
    gffn = np.ascontiguousarray(np.broadcast_to(f(norm_ffn_g)[0][None, :], (128, D)))
    cst, strips = _const_tables()
    shared = {"w_in": wi, "w_out": f(w_out)[0], "pvec": pvec, "wgates": wgates, "wr": wr, "brow": brow,
              "gfin": gfin, "gffn": gffn, "cst": cst, "cfq": strips[0], "ctab": strips[1], "dmask": strips[2],
              "w_gate": f(w_gate)[0], "w_up": f(w_up)[0], "w_down": f(w_down)[0]}
    if "nc" not in _NC_CACHE:
        _NC_CACHE["nc"] = build_program()
    nc = _NC_CACHE["nc"]
    in_maps = [dict(shared, x=x[c]) for c in range(NCORES)]
    res = run_bass_kernel_spmd(nc, in_maps, core_ids=list(range(NCORES)))
    out = np.stack([np.asarray(r["out"], dtype=np.float32) for r in res.results], 0)
    return out.reshape(16, S, D)
```
